# Optimizing a Trainium2 kernel written in Bass

```python
import jax
import jax.numpy as jnp
from jax import lax
import numpy as np

D_MODEL = 1024
BATCH = 4
SEQ = 8192
DEPTH = 1

GM_GROUPS = 4
GM_DIM = 128
GM_WIDTH = GM_GROUPS * GM_DIM
GM_CHUNK = 128
GDN_HEADS = 4
GDN_DK = 128
GDN_DV = 128
GDN_QK_WIDTH = GDN_HEADS * GDN_DK
GDN_V_WIDTH = GDN_HEADS * GDN_DV
GDN_CONV = 4
GDN_CHUNK = 64
D_MIX = GM_WIDTH + GDN_V_WIDTH
IN_SPLITS = (GM_WIDTH, 2 * GM_WIDTH, 2 * GM_WIDTH + GDN_QK_WIDTH, 2 * GM_WIDTH + 2 * GDN_QK_WIDTH, 2 * GM_WIDTH + 2 * GDN_QK_WIDTH + GDN_V_WIDTH, 2 * GM_WIDTH + 2 * GDN_QK_WIDTH + 2 * GDN_V_WIDTH, 2 * GM_WIDTH + 2 * GDN_QK_WIDTH + 2 * GDN_V_WIDTH + GDN_HEADS)
IN_COLS = IN_SPLITS[-1] + GDN_HEADS
N_EXPERTS = 32
TOP_K = 4
D_FF = D_MODEL
SWIGLU_LIMIT = 7.0
SWIGLU_ALPHA = 1.702
MOE_BLOCK = 256
N_MOD = 6
EPS = 1e-6

kernel_name = 'hybrid_gmlp_gdn_moe_adaln'


def _rmsnorm(x, w):
    xf = x.astype(jnp.float32)
    y = xf * lax.rsqrt(jnp.mean(xf * xf, axis=-1, keepdims=True) + EPS)
    return y.astype(x.dtype) * w


def _l2norm(x):
    return x * lax.rsqrt(jnp.sum(x * x, axis=-1, keepdims=True) + EPS)


def _modulate(x, w, shift, scale):
    return _rmsnorm(x, w) * (1 + scale[:, None, :]) + shift[:, None, :]


def _chunked_gmlp(u, v, v_norm_w, w_spatial, b_spatial):
    bsz, seq = u.shape[:2]
    n_chunks = seq // GM_CHUNK
    u = jax.nn.gelu(u, approximate=False)
    v = _rmsnorm(jax.nn.gelu(v, approximate=False), v_norm_w)
    causal = jnp.tril(jnp.ones((GM_CHUNK, GM_CHUNK), dtype=bool))
    ws = jnp.where(causal[None], w_spatial, jnp.zeros_like(w_spatial))
    vc = v.reshape(bsz, n_chunks, GM_CHUNK, GM_GROUPS, GM_DIM)
    z = jnp.einsum('gts,bnsgd->bntgd', ws, vc) + b_spatial.T[None, None, :, :, None]
    return (u * z.reshape(bsz, seq, GM_GROUPS, GM_DIM)).reshape(bsz, seq, GM_WIDTH)


def _chunk_gated_delta(q, k, v, g, beta):
    bsz, seq, nh, dk = q.shape
    dv = v.shape[-1]
    n = seq // GDN_CHUNK

    def to_chunks(t):
        return t.reshape(bsz, n, GDN_CHUNK, nh, -1).transpose(0, 3, 1, 2, 4)

    q, k, v = to_chunks(q), to_chunks(k), to_chunks(v)
    gc = jnp.cumsum(g.reshape(bsz, n, GDN_CHUNK, nh).transpose(0, 3, 1, 2), axis=-1)
    beta = beta.reshape(bsz, n, GDN_CHUNK, nh).transpose(0, 3, 1, 2)[..., None]
    tri = jnp.tril(jnp.ones((GDN_CHUNK, GDN_CHUNK), dtype=bool))
    strict = jnp.tril(jnp.ones((GDN_CHUNK, GDN_CHUNK), dtype=bool), k=-1)
    decay = jnp.exp(jnp.where(tri, gc[..., :, None] - gc[..., None, :], -jnp.inf))
    kb = k * beta
    a_mat = jnp.where(strict, jnp.einsum('bhnid,bhnjd->bhnij', kb, k) * decay, 0.0)
    eye = jnp.eye(GDN_CHUNK, dtype=q.dtype)
    rhs = jnp.concatenate([v * beta, kb * jnp.exp(gc)[..., None]], axis=-1)
    sol = lax.linalg.triangular_solve(a_mat + eye, rhs, left_side=True, lower=True, unit_diagonal=True)
    u_c, w_c = sol[..., :dv], sol[..., dv:]
    qk = jnp.where(tri, jnp.einsum('bhnid,bhnjd->bhnij', q, k) * decay, 0.0)

    def step(state, xs):
        q_i, k_i, u_i, w_i, g_i, qk_i = xs
        v_new = u_i - jnp.einsum('bhcd,bhde->bhce', w_i, state)
        o_i = jnp.einsum('bhcd,bhde->bhce', q_i * jnp.exp(g_i)[..., None], state) + jnp.einsum('bhij,bhje->bhie', qk_i, v_new)
        g_last = g_i[..., -1]
        k_dec = k_i * jnp.exp(g_last[..., None] - g_i)[..., None]
        state = state * jnp.exp(g_last)[..., None, None] + jnp.einsum('bhcd,bhce->bhde', k_dec, v_new)
        return state, o_i

    def front(t):
        return jnp.moveaxis(t, 2, 0)

    state0 = jnp.zeros((bsz, nh, dk, dv), q.dtype)
    _, o = lax.scan(step, state0, (front(q), front(k), front(u_c), front(w_c), front(gc), front(qk)))
    return o.transpose(1, 0, 3, 2, 4).reshape(bsz, seq, nh, dv)


def _gated_deltanet(q, k, v, a, b, z, conv_w, a_log, dt_bias, o_norm_w):
    dtype = v.dtype
    bsz, seq, _ = q.shape
    qkv = jnp.concatenate([q, k, v], axis=-1)
    n_ch = qkv.shape[-1]
    qkv = lax.conv_general_dilated(qkv, conv_w[:, None, :], window_strides=(1,), padding=[(GDN_CONV - 1, 0)], dimension_numbers=('NWC', 'WIO', 'NWC'), feature_group_count=n_ch)
    qkv = jax.nn.silu(qkv).astype(jnp.float32)
    q, k, v = jnp.split(qkv, [GDN_QK_WIDTH, 2 * GDN_QK_WIDTH], axis=-1)
    q = _l2norm(q.reshape(bsz, seq, GDN_HEADS, GDN_DK)) * (GDN_DK ** -0.5)
    k = _l2norm(k.reshape(bsz, seq, GDN_HEADS, GDN_DK))
    v = v.reshape(bsz, seq, GDN_HEADS, GDN_DV)
    beta = jax.nn.sigmoid(b.astype(jnp.float32))
    g = -jnp.exp(a_log.astype(jnp.float32)) * jax.nn.softplus(a.astype(jnp.float32) + dt_bias.astype(jnp.float32))
    o = _chunk_gated_delta(q, k, v, g, beta).astype(dtype)
    o = _rmsnorm(o, o_norm_w) * jax.nn.silu(z.reshape(bsz, seq, GDN_HEADS, GDN_DV))
    return o.reshape(bsz, seq, GDN_V_WIDTH)


def _moe(h, w_router, b_router, w_gu, b_gu, w_down, b_down):
    bsz, seq, d = h.shape
    n_tok = bsz * seq
    hf = h.reshape(n_tok, d)
    logits = (hf @ w_router + b_router).astype(jnp.float32)
    top_val, top_idx = lax.top_k(logits, TOP_K)
    weights = jax.nn.softmax(top_val, axis=-1).astype(h.dtype)
    n_assign = n_tok * TOP_K
    e_flat = top_idx.reshape(-1)
    tok_flat = jnp.repeat(jnp.arange(n_tok, dtype=jnp.int32), TOP_K)
    w_flat = weights.reshape(-1)
    order = jnp.argsort(e_flat)
    e_sorted = e_flat[order]
    counts = jnp.bincount(e_flat, length=N_EXPERTS)
    padded = (counts + MOE_BLOCK - 1) // MOE_BLOCK * MOE_BLOCK
    start = jnp.cumsum(counts) - counts
    pad_end = jnp.cumsum(padded)
    pad_start = pad_end - padded
    dest = pad_start[e_sorted] + jnp.arange(n_assign, dtype=jnp.int32) - start[e_sorted]
    n_slots = n_assign + N_EXPERTS * MOE_BLOCK
    n_blocks = n_slots // MOE_BLOCK
    slot_tok = jnp.full((n_slots,), n_tok, jnp.int32).at[dest].set(tok_flat[order])
    slot_w = jnp.zeros((n_slots,), h.dtype).at[dest].set(w_flat[order])
    block_e = jnp.minimum(jnp.searchsorted(pad_end, jnp.arange(n_blocks, dtype=jnp.int32) * MOE_BLOCK, side='right'), N_EXPERTS - 1)
    h_pad = jnp.concatenate([hf, jnp.zeros((1, d), hf.dtype)], axis=0)

    def expert_block(args):
        tok, wt, e = args
        xb = h_pad[tok]
        gu = xb @ w_gu[e] + b_gu[e]
        gate = jnp.minimum(gu[:, :D_FF], SWIGLU_LIMIT)
        up = jnp.clip(gu[:, D_FF:], -SWIGLU_LIMIT, SWIGLU_LIMIT)
        act = gate * jax.nn.sigmoid(SWIGLU_ALPHA * gate) * (up + 1)
        return (act @ w_down[e] + b_down[e]) * wt[:, None]

    y = lax.map(expert_block, (slot_tok.reshape(n_blocks, MOE_BLOCK), slot_w.reshape(n_blocks, MOE_BLOCK), block_e))
    out = jax.ops.segment_sum(y.reshape(n_slots, d), slot_tok, num_segments=n_tok + 1)[:n_tok]
    return out.reshape(bsz, seq, d)


def setup_inputs(seed: int = 0) -> dict:
    key = jax.random.key(seed)
    ks = jax.random.split(key, 24)
    L, D = DEPTH, D_MODEL

    def nrm(k, shape, s):
        return s * jax.random.normal(k, shape, jnp.float32)

    return {
        'x': nrm(ks[0], (BATCH, SEQ, D), 1.0),
        'c': nrm(ks[1], (BATCH, D), 1.0),
        'w_ada': nrm(ks[2], (L, D, N_MOD * D), 0.5 * D ** -0.5),
        'b_ada': nrm(ks[3], (L, N_MOD * D), 0.02),
        'norm1_w': 1.0 + nrm(ks[4], (L, D), 0.05),
        'w_in': nrm(ks[5], (L, D, IN_COLS), D ** -0.5),
        'gm_vnorm_w': 1.0 + nrm(ks[6], (L, GM_GROUPS, GM_DIM), 0.05),
        'gm_w_spatial': nrm(ks[7], (L, GM_GROUPS, GM_CHUNK, GM_CHUNK), GM_CHUNK ** -0.5),
        'gm_b_spatial': 1.0 + nrm(ks[8], (L, GM_GROUPS, GM_CHUNK), 0.1),
        'gdn_conv_w': nrm(ks[9], (L, GDN_CONV, 2 * GDN_QK_WIDTH + GDN_V_WIDTH), GDN_CONV ** -0.5),
        'gdn_a_log': jnp.log(jax.random.uniform(ks[10], (L, GDN_HEADS), jnp.float32, 1.0, 16.0)),
        'gdn_dt_bias': jnp.log(jnp.expm1(jax.random.uniform(ks[11], (L, GDN_HEADS), jnp.float32, 1e-3, 1e-1))),
        'gdn_onorm_w': 1.0 + nrm(ks[12], (L, GDN_DV), 0.05),
        'w_out': nrm(ks[13], (L, D_MIX, D), D_MIX ** -0.5),
        'norm2_w': 1.0 + nrm(ks[14], (L, D), 0.05),
        'w_router': nrm(ks[15], (L, D, N_EXPERTS), D ** -0.5),
        'b_router': nrm(ks[16], (L, N_EXPERTS), 0.01),
        'w_gu': nrm(ks[17], (L, N_EXPERTS, D, 2 * D_FF), D ** -0.5),
        'b_gu': nrm(ks[18], (L, N_EXPERTS, 2 * D_FF), 0.02),
        'w_down': nrm(ks[19], (L, N_EXPERTS, D_FF, D), D_FF ** -0.5),
        'b_down': nrm(ks[20], (L, N_EXPERTS, D), 0.02),
        'norm_f_w': 1.0 + nrm(ks[21], (D,), 0.05),
    }


def reference(x, c, w_ada, b_ada, norm1_w, w_in, gm_vnorm_w, gm_w_spatial, gm_b_spatial, gdn_conv_w, gdn_a_log, gdn_dt_bias, gdn_onorm_w, w_out, norm2_w, w_router, b_router, w_gu, b_gu, w_down, b_down, norm_f_w):
    bsz, seq, _ = x.shape
    c_act = jax.nn.silu(c)
    for l in range(DEPTH):
        mod = c_act @ w_ada[l] + b_ada[l]
        sh1, sc1, g1, sh2, sc2, g2 = jnp.split(mod, N_MOD, axis=-1)
        h = _modulate(x, norm1_w[l], sh1, sc1)
        proj = h @ w_in[l]
        gm_u, gm_v, q, k, v, z, a, b = jnp.split(proj, IN_SPLITS, axis=-1)
        y_a = _chunked_gmlp(gm_u.reshape(bsz, seq, GM_GROUPS, GM_DIM), gm_v.reshape(bsz, seq, GM_GROUPS, GM_DIM), gm_vnorm_w[l], gm_w_spatial[l], gm_b_spatial[l])
        y_b = _gated_deltanet(q, k, v, a, b, z, gdn_conv_w[l], gdn_a_log[l], gdn_dt_bias[l], gdn_onorm_w[l])
        mix = jnp.concatenate([y_a, y_b], axis=-1) @ w_out[l]
        x = x + g1[:, None, :] * mix
        h = _modulate(x, norm2_w[l], sh2, sc2)
        x = x + g2[:, None, :] * _moe(h, w_router[l], b_router[l], w_gu[l], b_gu[l], w_down[l], b_down[l])
    return _rmsnorm(x, norm_f_w)
```

```python
from contextlib import ExitStack
import numpy as np
import concourse.bass as bass
import concourse.mybir as mybir
from concourse.bass_utils import run_bass_kernel_spmd

F32 = mybir.dt.float32
BF16 = mybir.dt.bfloat16
AF = mybir.ActivationFunctionType
ALU = mybir.AluOpType
AX = mybir.AxisListType

D = 1024
NCOLS = 3080
NE = 32
EPS = 1e-6


class Buf:
    __slots__ = ("name", "w", "r", "excl")

    def __init__(self, name, excl=False):
        self.name = name
        self.w = {}
        self.r = {}
        self.excl = excl


class V:
    __slots__ = ("ap", "bufs")

    def __init__(self, ap, bufs):
        self.ap = ap
        self.bufs = bufs

    def __getitem__(self, key):
        return V(self.ap[key], self.bufs)

    def re(self, s, **kw):
        return V(self.ap.rearrange(s, **kw), self.bufs)

    def bc(self, shape):
        return V(self.ap.to_broadcast(shape), self.bufs)


class Op:
    __slots__ = ("eng", "fn", "deps", "idx", "inc", "val", "dkey", "cond", "dn", "always")


COMPUTE = ("pe", "act", "dve", "pool")


class Prog:
    def __init__(self, nc):
        self.nc = nc
        self.ops = {e: [] for e in COMPUTE + ("sp",)}
        self.emitted = {e: 0 for e in COMPUTE + ("sp",)}
        self.dcount = {}
        self.sems = {}
        self.es = ExitStack()
        self.seen = {e: {} for e in COMPUTE + ("sp",)}
        self.ninc = {e: 0 for e in COMPUTE}
        self.barrier = None
        self.regs = {}
        self.cond = None
        self.always = False
        self.flags_ap = None

    def bc_reg(self, e, val):
        if val not in self.regs:
            reg = e.alloc_register("bcr%d" % val)
            e.reg_mov(reg, val)
            self.regs[val] = reg
        return self.regs[val]

    def sem(self, name):
        if name not in self.sems:
            self.sems[name] = self.es.enter_context(self.nc.semaphore("s_" + name.replace(":", "_")))
        return self.sems[name]

    def add(self, eng, fn, reads, writes, dkey=None):
        op = Op()
        op.eng = eng
        op.fn = fn
        op.inc = False
        op.val = None
        op.dkey = dkey
        op.cond = self.cond
        op.always = self.always
        op.dn = None
        op.idx = len(self.ops[eng])
        deps = {}

        def need(src, idx):
            if src == "pe" and eng == "pe" and dkey is None:
                return
            if deps.get(src, -1) < idx:
                deps[src] = idx

        rb = [b for v in reads if isinstance(v, V) for b in v.bufs]
        wb = [b for v in writes if isinstance(v, V) for b in v.bufs]
        for b in rb:
            for s, i in b.w.items():
                need(s, i)
            if b.excl:
                for s, i in b.r.items():
                    if s != eng:
                        need(s, i)
        for b in wb:
            for s, i in b.w.items():
                need(s, i)
            for s, i in b.r.items():
                need(s, i)
        op.deps = deps
        if dkey is not None:
            self.dcount[dkey] = self.dcount.get(dkey, 0) + 1
            op.dn = self.dcount[dkey]
            me = ("D:" + dkey, self.dcount[dkey])
        else:
            me = (eng, op.idx)
        for b in rb:
            b.r[me[0]] = me[1]
        for b in wb:
            b.w[me[0]] = me[1]
        self.ops[eng].append(op)
        return op

    def emit(self):
        nc = self.nc
        for e in self.ops:
            for op in self.ops[e][self.emitted[e]:]:
                for s, i in op.deps.items():
                    if not s.startswith("D:") and i >= self.emitted[s]:
                        self.ops[s][i].inc = True
        for e in COMPUTE:
            if len(self.ops[e]) > self.emitted[e]:
                self.ops[e][-1].inc = True
        for e in COMPUTE:
            for op in self.ops[e][self.emitted[e]:]:
                if op.inc and op.dkey is None:
                    self.ninc[e] += 1
                    op.val = self.ninc[e]
        engs = {"pe": nc.tensor, "act": nc.scalar, "dve": nc.vector, "pool": nc.gpsimd, "sp": nc.sync}
        for k in self.dcount:
            self.sem("D:" + k)
        for e in COMPUTE:
            self.sem(e)
        barrier = self.barrier
        start = dict(self.emitted)

        def run(ename, engine):
            seen = self.seen[ename]
            ops = self.ops[ename][self.emitted[ename]:]
            plan = []
            first = True
            for op in ops:
                waits = {}
                if first and barrier is not None:
                    for s, v in barrier.items():
                        waits[s] = v
                first = False
                for s, i in op.deps.items():
                    if s.startswith("D:"):
                        v = 16 * i
                    else:
                        if i < start[s]:
                            continue
                        v = self.ops[s][i].val
                        assert v is not None, (s, i)
                    if waits.get(s, 0) < v:
                        waits[s] = v
                wl = []
                for s, v in waits.items():
                    if seen.get(s, 0) >= v:
                        continue
                    seen[s] = v
                    wl.append((s, v))
                plan.append((op, wl))

            def emit_real(group):
                for op, wl in group:
                    for s, v in wl:
                        engine.wait_ge(self.sem(s), v)
                    ins = op.fn(engine)
                    if op.dkey is not None:
                        ins.then_inc(self.sem("D:" + op.dkey), 16)
                    elif op.inc:
                        ins.then_inc(self.sem(ename), 1)

            def emit_skip(group):
                pend_inc = {}
                pend_wait = {}

                def flush():
                    for k, (before, tot) in pend_inc.items():
                        engine.wait_ge(self.sem(k), before)
                        engine.sem_inc(self.sem(k), tot)
                    for s_, v in pend_wait.items():
                        engine.wait_ge(self.sem(s_), v)
                    pend_inc.clear()
                    pend_wait.clear()

                for op, wl in group:
                    if op.always:
                        flush()
                        emit_real([(op, wl)])
                        continue
                    if op.dkey is not None:
                        k = "D:" + op.dkey
                        if k not in pend_inc:
                            pend_inc[k] = [16 * (op.dn - 1), 0]
                        pend_inc[k][1] += 16
                    elif op.inc:
                        if ename not in pend_inc:
                            pend_inc[ename] = [op.val - 1, 0]
                        pend_inc[ename][1] += 1
                    for s_, v in wl:
                        if pend_wait.get(s_, 0) < v:
                            pend_wait[s_] = v
                flush()

            def tag_at(op, depth):
                c = op.cond
                if c is None or len(c) <= depth:
                    return None
                return c[depth]

            def emit_level(items, depth):
                i = 0
                while i < len(items):
                    tag = tag_at(items[i][0], depth)
                    j = i
                    while j < len(items) and tag_at(items[j][0], depth) == tag:
                        j += 1
                    group = items[i:j]
                    if tag is None:
                        emit_real(group)
                    else:
                        key = "cr_" + ename
                        if key not in self.regs:
                            self.regs[key] = engine.alloc_register(key)
                        reg = self.regs[key]
                        engine.reg_load(reg, self.flags_ap[0:1, tag:tag + 1])
                        with engine.If_eq(reg, 0):
                            emit_skip(group)
                        with engine.Else():
                            emit_level(group, depth + 1)
                    i = j

            emit_level(plan, 0)

        with nc.Block() as block:
            @block.tensor
            def _(eng):
                run("pe", eng)

            @block.scalar
            def _(eng):
                run("act", eng)

            @block.vector
            def _(eng):
                run("dve", eng)

            @block.gpsimd
            def _(eng):
                run("pool", eng)

            @block.sync
            def _(eng):
                run("sp", eng)
        for e in self.ops:
            self.emitted[e] = len(self.ops[e])
        b = {}
        for e in COMPUTE:
            if self.ninc[e]:
                b[e] = self.ninc[e]
        for k, c in self.dcount.items():
            b["D:" + k] = 16 * c
        self.barrier = b

    def final_wait(self):
        nc = self.nc
        b = self.barrier
        with nc.Block() as block:
            @block.sync
            def _(eng):
                for s, v in b.items():
                    eng.wait_ge(self.sem(s), v)
        self.es.close()

    def mm(self, out, lhsT, rhs, start=True, stop=True):
        rd = [lhsT, rhs] + ([] if start else [out])
        self.add("pe", lambda e: e.matmul(out.ap, lhsT.ap, rhs.ap, start=start, stop=stop), rd, [out])

    def tr(self, out, in_, ident):
        self.add("pe", lambda e: e.transpose(out.ap, in_.ap, ident.ap), [in_, ident], [out])

    def act(self, out, in_, func, bias=None, scale=1.0, accum=None, eng="act"):
        rd = [in_]
        kw = {}
        if bias is not None:
            kw["bias"] = bias.ap if isinstance(bias, V) else bias
            rd.append(bias)
        if isinstance(scale, V):
            kw["scale"] = scale.ap
            rd.append(scale)
        else:
            kw["scale"] = scale
        wr = [out]
        if accum is not None:
            kw["accum_out"] = accum.ap
            wr.append(accum)
        self.add(eng, lambda e: e.activation(out.ap, in_.ap, func, **kw), rd, wr)

    def tt(self, eng, out, a, b, op):
        self.add(eng, lambda e: e.tensor_tensor(out.ap, a.ap, b.ap, op), [a, b], [out])

    def ts(self, eng, out, a, s1, op0, s2=None, op1=None):
        rd = [a]
        x1 = s1.ap if isinstance(s1, V) else s1
        x2 = s2.ap if isinstance(s2, V) else s2
        if isinstance(s1, V):
            rd.append(s1)
        if isinstance(s2, V):
            rd.append(s2)
        if op1 is None:
            self.add(eng, lambda e: e.tensor_scalar(out.ap, a.ap, x1, None, op0), rd, [out])
        else:
            self.add(eng, lambda e: e.tensor_scalar(out.ap, a.ap, x1, x2, op0, op1), rd, [out])

    def stt(self, eng, out, in0, scalar, in1, op0, op1):
        rd = [in0, in1]
        sc = scalar.ap if isinstance(scalar, V) else scalar
        if isinstance(scalar, V):
            rd.append(scalar)
        self.add(eng, lambda e: e.scalar_tensor_tensor(out.ap, in0.ap, sc, in1.ap, op0, op1), rd, [out])

    def rsq(self, out, in_, scale=1.0):
        self.act(out, in_, AF.Ln, bias=EPS, scale=scale)
        self.act(out, out, AF.Exp, scale=-0.5)

    def cp(self, eng, out, in_):
        if eng == "act":
            self.add(eng, lambda e: e.copy(out.ap, in_.ap), [in_], [out])
        else:
            self.add(eng, lambda e: e.tensor_copy(out.ap, in_.ap), [in_], [out])

    def memset(self, eng, out, val):
        self.add(eng, lambda e: e.memset(out.ap, val), [], [out])

    def rsum(self, eng, out, in_):
        self.add(eng, lambda e: e.reduce_sum(out.ap, in_.ap, AX.X), [in_], [out])

    def dma(self, eng, out, in_, key, slow=False):
        if slow:
            self.add(eng, lambda e: e.dma_start(out.ap, in_.ap, allow_slow_non_contiguous=True), [in_], [out], dkey=key)
        else:
            self.add(eng, lambda e: e.dma_start(out.ap, in_.ap), [in_], [out], dkey=key)


def build(NPRE, NMAIN, debug=False, n_exp=NE, cap=2048, use_skip=True, PIPE=True):
    nc = bass.Bass("TRN2", target_bir_lowering=False)
    P = Prog(nc)
    NT = NPRE + NMAIN
    NTOK = NMAIN * 128

    def din(name, shape):
        t = nc.dram_tensor(name, list(shape), F32, kind="ExternalInput")
        return V(t.ap(), [Buf(name)])

    x_seq = din("x_seq", [NT * 128, D])
    flag_d = din("flag", [128, 1])
    c_row = din("c_row", [8, 128])
    consts_all = din("consts", [128, 512 + NE])
    consts = consts_all[:, 0:512]
    consts2 = consts_all[:, 512:512 + NE]
    w_ada = din("w_ada", [D, 6 * D])
    b_ada = din("b_ada", [1, 6 * D])
    norm1_w = din("norm1_w", [1, D])
    w_in = din("w_in", [D, NCOLS])
    gm_vnorm_w = din("gm_vnorm_w", [1, 512])
    gm_w_spatial = din("gm_w_spatial", [4, 128, 128])
    gm_b_spatial = din("gm_b_spatial", [4, 128])
    gdn_conv_w = din("gdn_conv_w", [4, 1536])
    gdn_a_log = din("gdn_a_log", [1, 4])
    gdn_dt_bias = din("gdn_dt_bias", [1, 4])
    gdn_onorm_w = din("gdn_onorm_w", [1, 128])
    w_out = din("w_out", [D, D])
    norm2_w = din("norm2_w", [1, D])
    w_router = din("w_router", [D, NE])
    b_router = din("b_router", [1, NE])
    w_gu = din("w_gu", [NE, D, 2 * D])
    b_gu = din("b_gu", [NE, 2 * D])
    w_down = din("w_down", [NE, D, D])
    b_down = din("b_down", [NE, D])
    norm_f_w = din("norm_f_w", [1, D])
    out_t = nc.dram_tensor("out", [NTOK, D], F32, kind="ExternalOutput")
    out_d = V(out_t.ap(), [Buf("out")])
    x1_t = nc.dram_tensor("x1s", [NTOK, D], F32, kind="Internal")
    x1_d = V(x1_t.ap(), [Buf("x1s")])
    dbg = {}
    if debug:
        for nm, shp in debug.items():
            t = nc.dram_tensor(nm, list(shp), F32, kind="ExternalOutput")
            dbg[nm] = V(t.ap(), [Buf(nm)])

    es = ExitStack()

    def sb(name, shape, dt=F32, st=None):
        t = (st or es).enter_context(nc.sbuf_tensor(name, list(shape), dt))
        return V(t[:], [Buf(name)])

    def ps(name, shape, dt=F32, st=None):
        t = (st or es).enter_context(nc.psum_tensor(name, list(shape), dt))
        return V(t[:], [Buf(name, excl=True)])

    def bcast_rows(src, n):
        return V(src.ap[0:1, :].to_broadcast([128, n]), src.bufs)

    cst = sb("cst", [128, 512])
    mod1 = sb("mod1", [128, 3 * D])
    identB = sb("identB", [128, 128], BF16)
    onesB = sb("onesB", [128, 128], BF16)
    flagT = sb("flagT", [128, 1])
    identF = cst[:, 0:128]
    triu = cst[:, 128:256]
    tril = cst[:, 256:384]
    stril = cst[:, 384:512]
    sh1 = mod1[:, 0:D]
    A1 = mod1[:, D:2 * D]
    g1R = mod1[:, 2 * D:3 * D]
    mod2_t = nc.dram_tensor("mod2s", [128, 3 * D], F32, kind="Internal")
    mod2_d = V(mod2_t.ap(), [Buf("mod2s")])

    P.dma("sp", cst, consts, "cst")
    P.dma("sp", flagT, flag_d, "flag")
    P.cp("dve", identB, identF)
    P.memset("dve", onesB, 1.0)

    pb = [ps("pb%d" % i, [128, 512]) for i in range(7)]
    ptp = ps("ptp", [128, 1024], BF16)

    s1 = ExitStack()

    def sb1(name, shape, dt=F32):
        return sb(name, shape, dt, st=s1)

    w_inB = sb1("w_inB", [128, 8, NCOLS], BF16)
    w_in_v = w_in.re("(k p) n -> p k n", p=128)
    for k in range(8):
        P.dma("pool", w_inB[:, k, :], w_in_v[:, k, :], "w_inB")
    w_outB = sb1("w_outB", [128, 8, D], BF16)
    w_out_v = w_out.re("(k p) n -> p k n", p=128)
    for k in range(0, 8, 4):
        P.dma("pool", w_outB[:, k:k + 4, :], w_out_v[:, k:k + 4, :], "w_outB")
    s0 = ExitStack()

    def sb0(name, shape, dt=F32):
        return sb(name, shape, dt, st=s0)

    modR = sb0("modR", [128, 6 * D])
    A1t = modR[:, D:2 * D]
    A2t = modR[:, 4 * D:5 * D]
    cT = sb0("cT", [128, 8])
    cact = sb0("cact", [128, 8])
    cactB = sb0("cactB", [128, 8, 128])
    P.dma("sp", cT, c_row.re("k p -> p k"), "cT", slow=True)
    P.act(cact, cT, AF.Silu)
    for k in range(8):
        P.cp("dve", cactB[:, k, :], cact[:, k:k + 1].bc([128, 128]))
    P.dma("sp", modR, bcast_rows(b_ada, 6 * D), "modR")
    wst = [sb0("wst%d" % i, [128, 8, 512]) for i in range(2)]
    w_ada_v = w_ada.re("(k p) n -> p k n", p=128)
    for cb in range(12):
        st = wst[cb % 2]
        P.dma("sp", st, w_ada_v[:, :, cb * 512:(cb + 1) * 512], "wst%d" % (cb % 2))
        pm = pb[cb % 2]
        for k in range(8):
            P.mm(pm, cactB[:, k, :], st[:, k, :], start=(k == 0), stop=(k == 7))
        P.tt("dve", modR[:, cb * 512:(cb + 1) * 512], modR[:, cb * 512:(cb + 1) * 512], pm, ALU.add)
    nwR = sb0("nwR", [128, D])
    P.dma("sp", nwR, bcast_rows(norm1_w, D), "nwR")
    P.stt("dve", A1t, A1t, 1.0, nwR, ALU.add, ALU.mult)
    nwR2 = sb0("nwR2", [128, D])
    P.dma("sp", nwR2, bcast_rows(norm2_w, D), "nwR2")
    P.stt("dve", A2t, A2t, 1.0, nwR2, ALU.add, ALU.mult)
    P.cp("act", mod1, modR[:, 0:3 * D])
    P.dma("sp", mod2_d, modR[:, 3 * D:6 * D], "mod2s")
    P.emit()
    s0.close()
    import os
    KSTOP = int(os.environ.get("KSTOP", "9"))
    KCUT = int(os.environ.get("KCUT", "99"))
    KSUB = int(os.environ.get("KSUB", "99"))
    if KSTOP == 0:
        P.final_wait()
        return nc
    if KSTOP == 1:
        NT = 0

    wsF = sb1("wsF", [128, 4, 128])
    P.dma("sp", wsF, gm_w_spatial.re("g t s -> t g s"), "wsF")
    for g in range(4):
        P.tt("dve", wsF[:, g, :], wsF[:, g, :], tril, ALU.mult)
    wsT = sb1("wsT", [128, 4, 128], BF16)
    for g in range(4):
        P.tr(pb[2][:, g * 128:(g + 1) * 128], wsF[:, g, :], identF)
    P.cp("dve", wsT.re("p g t -> p (g t)"), pb[2])
    bsp = sb1("bsp", [128, 4])
    P.dma("sp", bsp, gm_b_spatial.re("g t -> t g"), "bsp", slow=True)
    vnwR = sb1("vnwR", [128, 512])
    P.dma("sp", vnwR, bcast_rows(gm_vnorm_w, 512), "vnwR")
    convw = sb1("convw", [128, 4, 12])
    for j in range(4):
        P.dma("sp", convw[:, j, :], gdn_conv_w[j, :].re("(c p) -> p c", p=128), "convw", slow=True)
    alog = sb1("alog", [128, 4])
    dtb = sb1("dtb", [128, 4])
    nexpA = sb1("nexpA", [128, 4])
    P.dma("sp", alog, bcast_rows(gdn_a_log, 4), "alog")
    P.dma("sp", dtb, bcast_rows(gdn_dt_bias, 4), "dtb")
    P.act(nexpA, alog, AF.Exp)
    P.ts("dve", nexpA, nexpA, -1.0, ALU.mult)
    onwR = sb1("onwR", [128, 128])
    P.dma("sp", onwR, bcast_rows(gdn_onorm_w, 128), "onwR")

    xt = [sb1("xt%d" % i, [128, D]) for i in range(2)]
    ssq = sb1("ssq", [128, 1])
    rstd = sb1("rstd", [128, 1])
    hf = sb1("hf", [128, D])
    junk = hf
    hb = sb1("hb", [128, D], BF16)
    hT = sb1("hT", [128, D], BF16)
    uS = sb1("uS", [128, 512])
    gvS = sb1("gvS", [128, 512])
    vss = sb1("vss", [128, 4])
    vr = sb1("vr", [128, 4])
    vn = sb1("vn", [128, 512], BF16)
    ycat = [sb1("ycat%d" % i, [128, D], BF16) for i in range(2)]
    yT = sb1("yT", [128, D], BF16)
    cbuf = sb1("cbuf", [128, 12, 131])
    cvaA = sb1("cvaA", [128, 8, 128])
    cvaB = sb1("cvaB", [128, 4, 128])
    cvbA = sb1("cvbA", [128, 8, 128])
    cvbB = sb1("cvbB", [128, 4, 128])
    qkvS = sb1("qkvS", [128, 12, 128])
    sqn = sb1("sqn", [128, 8, 128], BF16)
    sq5 = cvaA[:, 0:4, :].re("p h e -> p (h e)")
    rn = cvbA
    t1 = cvbB
    Din = cvaA[:, 4:8, :]
    DTin = cvaA[:, 4:8, :]
    Dm = cvaB
    qT = sb1("qT", [128, 4, 128], BF16)
    kT = sb1("kT", [128, 4, 128], BF16)
    vTb = sb1("vTb", [128, 4, 128], BF16)
    abx = sb1("abx", [128, 4])
    aex = sb1("aex", [128, 4])
    asp = sb1("asp", [128, 4])
    gS = sb1("gS", [128, 4])
    beta = sb1("beta", [128, 4])
    nbeta = sb1("nbeta", [128, 4])
    gcS = sb1("gcS", [128, 4])
    egct = sb1("egct", [128, 4])
    bg = sb1("bg", [128, 4])
    dlin = sb1("dlin", [128, 4])
    dl = sb1("dl", [128, 4])
    gB = sb1("gB", [128, 4, 128])
    egcB = sb1("egcB", [128, 4, 128])
    DTm = sb1("DTm", [128, 4, 128])
    N0 = [sb1("N0_%d" % i, [128, 4, 128]) for i in range(2)]
    NT0 = [sb1("NT0_%d" % i, [128, 4, 128]) for i in range(2)]
    R0 = [sb1("R0_%d" % i, [128, 4, 128]) for i in range(2)]
    vb = [sb1("vb%d" % i, [128, 4, 128], BF16) for i in range(2)]
    kbg = [sb1("kbg%d" % i, [128, 4, 128], BF16) for i in range(2)]
    kdec = [sb1("kdec%d" % i, [128, 4, 128], BF16) for i in range(2)]
    qgT = [sb1("qgT%d" % i, [128, 4, 128], BF16) for i in range(2)]
    qkTb = [sb1("qkTb%d" % i, [128, 4, 128], BF16) for i in range(2)]
    egl = [sb1("egl%d" % i, [128, 4]) for i in range(2)]
    gz = [sb1("gz%d" % i, [128, 4, 128]) for i in range(2)]
    Pm = sb1("Pm", [128, 4, 128])
    PTm = sb1("PTm", [128, 4, 128])
    RT = sb1("RT", [128, 4, 128])
    TTb = sb1("TTb", [128, 4, 128], BF16)
    uU = sb1("uU", [128, 4, 128])
    osq = uU
    wTb = sb1("wTb", [128, 4, 128], BF16)
    vnew = sb1("vnew", [128, 4, 128], BF16)
    Sf = sb1("Sf", [128, 4, 128])
    Sb = sb1("Sb", [128, 4, 128], BF16)
    szS = sb1("szS", [128, 512])
    oss = sb1("oss", [128, 4])
    orr = sb1("orr", [128, 4])
    x1t = sb1("x1t0", [128, D])

    P.memset("dve", cbuf, 0.0)
    P.memset("dve", Sf, 0.0)
    P.memset("dve", Sb, 0.0)

    def f3(v):
        return v.re("p (h e) -> p h e", h=4)

    def f2(v):
        return v.re("p h e -> p (h e)")

    def gmlp(t):
        p = t % 2
        sq5g = szS
        yield
        yield
        yield

        def proj1(c0):
            for k in range(8):
                P.mm(pb[1], hT[:, k * 128:(k + 1) * 128], w_inB[:, k, c0:c0 + 512], start=(k == 0), stop=(k == 7))

        proj1(512)
        yield
        P.act(gvS, pb[1], AF.Gelu)
        yield
        proj1(0)
        P.tt("pool", sq5g, gvS, gvS, ALU.mult)
        yield
        P.act(uS, pb[1], AF.Gelu)
        P.rsum("dve", vss, f3(sq5g))
        yield
        P.rsq(vr, vss, 1.0 / 128)
        yield
        for g in range(4):
            P.stt("dve", vn[:, g * 128:(g + 1) * 128], gvS[:, g * 128:(g + 1) * 128], vr[:, g:g + 1],
                  vnwR[:, g * 128:(g + 1) * 128], ALU.mult, ALU.mult)
            if g % 2 == 1:
                yield
        for g in range(4):
            P.mm(pb[1][:, g * 128:(g + 1) * 128], wsT[:, g, :], vn[:, g * 128:(g + 1) * 128])
        yield
        for g in range(4):
            P.stt("dve", ycat[p][:, g * 128:(g + 1) * 128], pb[1][:, g * 128:(g + 1) * 128], bsp[:, g:g + 1],
                  uS[:, g * 128:(g + 1) * 128], ALU.add, ALU.mult)
            if g % 2 == 1:
                yield
        proj1(2560)
        yield
        P.act(szS, pb[1], AF.Silu)
        yield
        P.tt("pool", gz[p], f3(szS), onwR[:, None, :].bc([128, 4, 128]), ALU.mult)
        yield

    def stage1(t):
        main = t >= NPRE
        p = t % 2
        xs = xt[p]
        P.dma("sp", xs, x_seq[t * 128:(t + 1) * 128, :], "xt%d" % p)
        P.memset("dve", ssq, 0.0)
        P.act(junk, xs, AF.Square, accum=ssq)
        P.rsq(rstd, ssq, 1.0 / D)
        yield
        P.stt("dve", hf, xs, rstd, A1, ALU.mult, ALU.mult)
        P.tt("dve", hb, hf, sh1, ALU.add)
        yield
        for k in range(8):
            P.tr(ptp[:, k * 128:(k + 1) * 128], hb[:, k * 128:(k + 1) * 128], identB)
        P.cp("act", hT, ptp)
        yield

        def proj_tok(pso, c0, n):
            for k in range(8):
                P.mm(pso, hT[:, k * 128:(k + 1) * 128], w_inB[:, k, c0:c0 + n], start=(k == 0), stop=(k == 7))

        g0 = 0 if (main or t == NPRE - 1) else 1
        for grp in range(g0, 3):
            pq = (pb[0], pb[4], pb[5])[grp]
            for c in range(4):
                col = 1024 + (grp * 4 + c) * 128
                for k in range(8):
                    P.mm(pq[:, c * 128:(c + 1) * 128], w_inB[:, k, col:col + 128], hT[:, k * 128:(k + 1) * 128],
                         start=(k == 0), stop=(k == 7))
            P.cp("act", cbuf[:, grp * 4:(grp + 1) * 4, 3:131], f3(pq))
            yield
        for k in range(8):
            P.mm(pb[4][:, 0:8], hT[:, k * 128:(k + 1) * 128], w_inB[:, k, 3072:3080], start=(k == 0), stop=(k == 7))
        c0 = 0 if main else 4

        def cwA(j):
            return convw[:, j, c0:8, None].bc([128, 8 - c0, 128])

        def cwB(j):
            return convw[:, j, 8:12, None].bc([128, 4, 128])

        P.tt("dve", cvaA[:, c0:8, :], cbuf[:, c0:8, 0:128], cwA(0), ALU.mult)
        P.tt("pool", cvaB, cbuf[:, 8:12, 0:128], cwB(0), ALU.mult)
        P.tt("dve", cvbA[:, c0:8, :], cbuf[:, c0:8, 1:129], cwA(1), ALU.mult)
        P.tt("pool", cvbB, cbuf[:, 8:12, 1:129], cwB(1), ALU.mult)
        yield
        P.tt("dve", cvaA[:, c0:8, :], cvaA[:, c0:8, :], cvbA[:, c0:8, :], ALU.add)
        P.tt("pool", cvaB, cvaB, cvbB, ALU.add)
        P.tt("dve", cvbA[:, c0:8, :], cbuf[:, c0:8, 2:130], cwA(2), ALU.mult)
        P.tt("pool", cvbB, cbuf[:, 8:12, 2:130], cwB(2), ALU.mult)
        yield
        P.tt("dve", cvaA[:, c0:8, :], cvaA[:, c0:8, :], cvbA[:, c0:8, :], ALU.add)
        P.tt("pool", cvaB, cvaB, cvbB, ALU.add)
        P.tt("dve", cvbA[:, c0:8, :], cbuf[:, c0:8, 3:131], cwA(3), ALU.mult)
        P.tt("pool", cvbB, cbuf[:, 8:12, 3:131], cwB(3), ALU.mult)
        yield
        P.tt("dve", cvaA[:, c0:8, :], cvaA[:, c0:8, :], cvbA[:, c0:8, :], ALU.add)
        P.tt("pool", cvaB, cvaB, cvbB, ALU.add)
        P.cp("act", cbuf[:, :, 0:3], cbuf[:, :, 128:131])
        if t == NPRE - 1:
            P.ts("dve", cbuf[:, :, 0:3], cbuf[:, :, 0:3], flagT[:, 0:1], ALU.mult)
        P.act(qkvS[:, c0:8, :], cvaA[:, c0:8, :], AF.Silu)
        P.act(qkvS[:, 8:12, :], cvaB, AF.Silu)
        yield
        P.act(beta, pb[4][:, 4:8], AF.Sigmoid)
        P.tt("dve", abx, pb[4][:, 0:4], dtb, ALU.add)
        P.act(aex, abx, AF.Exp)
        P.act(asp, aex, AF.Ln, bias=1.0)
        yield
        P.tt("dve", sqn[:, c0:8, :], qkvS[:, c0:8, :], qkvS[:, c0:8, :], ALU.mult)
        P.tt("dve", gS, asp, nexpA, ALU.mult)
        P.ts("dve", nbeta, beta, -1.0, ALU.mult)
        yield
        if main:
            P.mm(pb[0], onesB, f2(sqn[:, 0:4, :]))
        P.mm(pb[5], onesB, f2(sqn[:, 4:8, :]))
        P.mm(pb[4][:, 8:12], triu, gS)
        P.cp("act", gB, gS[:, :, None].bc([128, 4, 128]))
        yield
        if main:
            P.rsq(f2(rn[:, 0:4, :]), pb[0])
        P.rsq(f2(rn[:, 4:8, :]), pb[5])
        P.cp("dve", gcS, pb[4][:, 8:12])
        for h in range(4):
            P.mm(pb[5][:, h * 128:(h + 1) * 128], gB[:, h, :], triu)
        yield
        if main:
            P.stt("dve", qT, qkvS[:, 0:4, :], float(128 ** -0.5), rn[:, 0:4, :], ALU.mult, ALU.mult)
        P.tt("dve", kT, qkvS[:, 4:8, :], rn[:, 4:8, :], ALU.mult)
        P.cp("act", vTb, qkvS[:, 8:12, :])
        P.act(f2(egcB), pb[5], AF.Exp)
        P.act(egct, gcS, AF.Exp)
        yield
        for h in range(4):
            P.tr(ptp[:, h * 128:(h + 1) * 128], kT[:, h, :], identB)
        for h in range(4):
            P.tr(ptp[:, 512 + h * 128:512 + (h + 1) * 128], vTb[:, h, :], identB)
        P.tt("dve", bg, beta, egct, ALU.mult)
        P.tt("dve", dlin, f3(pb[5])[:, :, 127], gcS, ALU.subtract)
        P.act(dl, dlin, AF.Exp)
        yield
        for h in range(4):
            P.mm(pb[4][:, h * 128:(h + 1) * 128], kT[:, h, :], kT[:, h, :])
        for h in range(4):
            P.ts("dve", Din[:, h, :], pb[5][:, h * 128:(h + 1) * 128], gcS[:, h:h + 1], ALU.subtract, 0.0, ALU.max)
        P.act(Dm, Din, AF.Exp, scale=-1.0)
        P.tt("pool", Dm, Dm, stril[:, None, :].bc([128, 4, 128]), ALU.mult)
        yield
        ktok = f3(ptp[:, 0:512])
        vtok = f3(ptp[:, 512:1024])
        P.tt("dve", kbg[p], ktok, bg[:, :, None].bc([128, 4, 128]), ALU.mult)
        P.tt("dve", kdec[p], ktok, dl[:, :, None].bc([128, 4, 128]), ALU.mult)
        P.tt("dve", vb[p], vtok, beta[:, :, None].bc([128, 4, 128]), ALU.mult)
        P.cp("act", egl[p], egcB[:, :, 127])
        yield
        P.tt("dve", t1, f3(pb[4]), Dm, ALU.mult)
        P.tt("dve", N0[p], t1, nbeta[:, :, None].bc([128, 4, 128]), ALU.mult)
        yield
        if main:
            for h in range(4):
                P.ts("dve", DTin[:, h, :], pb[5][:, h * 128:(h + 1) * 128], gcS[:, h:h + 1], ALU.subtract, 0.0, ALU.min)
            P.act(DTm, DTin, AF.Exp)
        yield
        for h in range(4):
            P.tr(pb[0][:, h * 128:(h + 1) * 128], N0[p][:, h, :], identF)
        if main:
            P.tt("pool", DTm, DTm, triu[:, None, :].bc([128, 4, 128]), ALU.mult)
            for h in range(4):
                P.mm(pb[4][:, h * 128:(h + 1) * 128], kT[:, h, :], qT[:, h, :])
        yield
        P.cp("act", f2(NT0[p]), pb[0])
        P.tt("dve", R0[p], f3(pb[0]), identF[:, None, :].bc([128, 4, 128]), ALU.add)
        if main:
            P.tt("pool", qgT[p], qT, egcB, ALU.mult)
            P.tt("dve", qkTb[p], f3(pb[4]), DTm, ALU.mult)
        yield

    def stage2(t):
        main = t >= NPRE
        p = t % 2
        xs = xt[p]
        for lvl in range(1, 7):
            last = lvl == 6
            Pc = N0[p] if lvl == 1 else Pm
            PTc = NT0[p] if lvl == 1 else PTm
            Rc = R0[p] if lvl == 1 else RT
            for h in range(4):
                P.mm(pb[2][:, h * 128:(h + 1) * 128], PTc[:, h, :], Pc[:, h, :])
            if not last:
                for h in range(4):
                    P.mm(pb[3][:, h * 128:(h + 1) * 128], Pc[:, h, :], PTc[:, h, :])
            P.cp("act", f2(Pm), pb[2])
            if not last:
                P.cp("dve", f2(PTm), pb[3])
            yield
            for h in range(4):
                P.mm(pb[6][:, h * 128:(h + 1) * 128], Pm[:, h, :], Rc[:, h, :])
            P.tt("dve", f2(RT), f2(Rc), pb[6], ALU.add)
            yield
        P.cp("act", TTb, RT)
        yield
        for h in range(4):
            P.mm(pb[2][:, h * 128:(h + 1) * 128], TTb[:, h, :], vb[p][:, h, :])
        for h in range(4):
            P.mm(pb[3][:, h * 128:(h + 1) * 128], kbg[p][:, h, :], TTb[:, h, :])
        yield
        P.cp("act", f2(uU), pb[2])
        P.cp("act", f2(wTb), pb[3])
        yield
        for h in range(4):
            P.mm(pb[6][:, h * 128:(h + 1) * 128], wTb[:, h, :], Sb[:, h, :])
        yield
        P.tt("dve", f2(vnew), f2(uU), pb[6], ALU.subtract)
        yield
        if main:
            for h in range(4):
                P.mm(pb[3][:, h * 128:(h + 1) * 128], qgT[p][:, h, :], Sb[:, h, :], start=True, stop=False)
                P.mm(pb[3][:, h * 128:(h + 1) * 128], qkTb[p][:, h, :], vnew[:, h, :], start=False, stop=True)
        for h in range(4):
            P.mm(pb[2][:, h * 128:(h + 1) * 128], kdec[p][:, h, :], vnew[:, h, :])
        yield
        for h in range(4):
            P.stt("dve", Sf[:, h, :], Sf[:, h, :], egl[p][:, h:h + 1], pb[2][:, h * 128:(h + 1) * 128], ALU.mult, ALU.add)
        if t == NPRE - 1:
            P.ts("dve", f2(Sf), f2(Sf), flagT[:, 0:1], ALU.mult)
        P.cp("act", Sb, Sf)
        yield
        if main:
            P.act(f2(osq), pb[3], AF.Square)
            yield
            P.rsum("dve", oss, osq)
            P.rsq(orr, oss, 1.0 / 128)
            yield
            for h in range(4):
                P.stt("dve", ycat[p][:, 512 + h * 128:512 + (h + 1) * 128], pb[3][:, h * 128:(h + 1) * 128],
                      orr[:, h:h + 1], gz[p][:, h, :], ALU.mult, ALU.mult)
            yield
            for k in range(8):
                P.tr(ptp[:, k * 128:(k + 1) * 128], ycat[p][:, k * 128:(k + 1) * 128], identB)
            yield
            P.cp("act", yT, ptp)
            yield
            xo = x1t
            for half in range(2):
                pbo = pb[2 + half]
                for k in range(8):
                    P.mm(pbo, yT[:, k * 128:(k + 1) * 128], w_outB[:, k, half * 512:(half + 1) * 512],
                         start=(k == 0), stop=(k == 7))
            yield
            for half in range(2):
                P.tt("dve", xo[:, half * 512:(half + 1) * 512], pb[2 + half], g1R[:, half * 512:(half + 1) * 512], ALU.mult)
            yield
            P.tt("dve", xo, xo, xs, ALU.add)
            m = t - NPRE
            P.dma("sp", x1_d[m * 128:(m + 1) * 128, :], xo, "x1st")
            if "d_x1" in dbg:
                P.dma("sp", dbg["d_x1"][m * 128:(m + 1) * 128, :], xo, "dbg")
            if "d_ycat" in dbg:
                P.cp("dve", hf, ycat[p])
                P.dma("sp", dbg["d_ycat"][m * 128:(m + 1) * 128, :], hf, "dbg")
            yield

    def interleave(*gs):
        gens = [g for g in gs if g is not None]
        while gens:
            for g in list(gens):
                try:
                    next(g)
                except StopIteration:
                    gens.remove(g)

    if PIPE:
        for t in range(NT + 1):
            interleave(stage1(t) if t < NT else None,
                       gmlp(t) if (t < NT and t >= NPRE) else None,
                       stage2(t - 1) if t >= 1 else None)
    else:
        for t in range(NT):
            for _ in stage1(t):
                pass
            if t >= NPRE:
                for _ in gmlp(t):
                    pass
            for _ in stage2(t):
                pass

    P.emit()
    s1.close()
    if KSTOP <= 2:
        P.final_wait()
        return nc

    C = cap
    NSLOT = NE * C
    BIG = 1.0e6
    xg_t = nc.dram_tensor("xg", [NSLOT, D], BF16, kind="Internal")
    xg_d = V(xg_t.ap(), [Buf("xg")])
    yy_t = nc.dram_tensor("yy", [NSLOT, D], BF16, kind="Internal")
    yy_d = V(yy_t.ap(), [Buf("yy")])
    I32 = mybir.dt.int32
    U32 = mybir.dt.uint32

    s2p = ExitStack()
    slotI = sb("slotI", [128, NMAIN, 4], I32, st=s2p)
    wk = sb("wk", [128, NMAIN, 4], F32, st=s2p)

    s2 = ExitStack()

    def sb2(name, shape, dt=F32):
        return sb(name, shape, dt, st=s2)

    mod2a = sb2("mod2a", [128, 2 * D])
    P.dma("sp", mod2a, mod2_d[:, 0:2 * D], "mod2a")
    sh2 = mod2a[:, 0:D]
    A2 = mod2a[:, D:2 * D]
    wrF = sb2("wrF", [128, 8, NE])
    P.dma("sp", wrF, w_router.re("(k p) n -> p k n", p=128), "wrF")
    brR = sb2("brR", [128, NE])
    P.dma("sp", brR, bcast_rows(b_router, NE), "brR")
    iotaE = sb2("iotaE", [128, NE])
    P.dma("sp", iotaE, consts2, "iotaE")
    striuB = sb2("striuB", [128, 128], BF16)
    P.tt("dve", striuB, triu, identF, ALU.subtract)
    maskacc = sb2("maskacc", [128, NE], BF16)
    P.memset("dve", maskacc, 0.0)
    def pp(name, shape, dt=F32):
        return [sb2("%s_%d" % (name, i), shape, dt) for i in range(2)]

    slotI_t = [V(slotI.ap[:, s_, :], [Buf("slotI%d" % s_)]) for s_ in range(NMAIN)]
    wk_t = [V(wk.ap[:, s_, :], [Buf("wk%d" % s_)]) for s_ in range(NMAIN)]
    x2 = pp("x2", [128, D])
    h2f_ = pp("h2f", [128, D])
    junk2_ = pp("junk2", [128, D])
    h2b = pp("h2b", [128, D], BF16)
    h2Tf_ = pp("h2Tf", [128, 8, 128])
    ssq2_ = pp("ssq2", [128, 1])
    rstd2_ = pp("rstd2", [128, 1])
    lg_ = pp("lg", [128, NE])
    top8_ = pp("top8", [128, 8])
    idx8_ = pp("idx8", [128, 8], U32)
    ekf_ = pp("ekf", [128, 4])
    msk_ = pp("msk", [128, NE], BF16)
    posS_ = pp("posS", [128, NE])
    tmp32_ = pp("tmp32", [128, NE])
    pk_ = pp("pk", [128, 4])
    val_ = pp("val", [128, 4])
    slotf_ = pp("slotf", [128, 4])
    ex4_ = pp("ex4", [128, 4])
    nmx_ = pp("nmx", [128, 1])
    den_ = pp("den", [128, 1])

    def tile2a(s):
        q = s % 2
        xs, hb2, h2f, junk2, h2Tf = x2[q], h2b[q], h2f_[q], junk2_[q], h2Tf_[q]
        ssq2, rstd2, lg, top8, idx8, ekf, msk = ssq2_[q], rstd2_[q], lg_[q], top8_[q], idx8_[q], ekf_[q], msk_[q]
        posS, tmp32, pk, val, slotf, ex4, nmx, den = posS_[q], tmp32_[q], pk_[q], val_[q], slotf_[q], ex4_[q], nmx_[q], den_[q]
        pA, pB, pC = (pb[0], pb[1], pb[2]) if q == 0 else (pb[4], pb[5], pb[6])
        sI, wkt = slotI_t[s], wk_t[s]
        P.dma("sp", xs, x1_d[s * 128:(s + 1) * 128, :], "x2_%d" % q)
        P.memset("dve", ssq2, 0.0)
        P.act(junk2, xs, AF.Square, accum=ssq2)
        yield
        P.rsq(rstd2, ssq2, 1.0 / D)
        yield
        P.stt("dve", h2f, xs, rstd2, A2, ALU.mult, ALU.mult)
        yield
        P.tt("dve", h2f, h2f, sh2, ALU.add)
        yield
        P.cp("act", hb2, h2f)
        for half, pbh in enumerate((pA, pB)):
            for k in range(4):
                kk = half * 4 + k
                P.tr(pbh[:, k * 128:(k + 1) * 128], h2f[:, kk * 128:(kk + 1) * 128], identF)
        yield
        P.cp("act", h2Tf[:, 0:4, :], f3(pA))
        P.cp("dve", h2Tf[:, 4:8, :], f3(pB))
        yield
        for k in range(8):
            P.mm(pC[:, 0:NE], h2Tf[:, k, :], wrF[:, k, :], start=(k == 0), stop=(k == 7))
        yield
        P.tt("dve", lg, pC[:, 0:NE], brR, ALU.add)
        yield
        P.add("dve", lambda e: e.max(out=top8.ap, in_=lg.ap), [lg], [top8])
        yield
        P.add("dve", lambda e: e.max_index(out=idx8.ap, in_max=top8.ap, in_values=lg.ap), [lg, top8], [idx8])
        P.ts("dve", msk, lg, top8[:, 3:4], ALU.is_ge)
        P.ts("dve", nmx, top8[:, 0:1], -1.0, ALU.mult)
        yield
        P.mm(pC[:, NE:2 * NE], striuB, msk, start=True, stop=False)
        P.mm(pC[:, NE:2 * NE], onesB, maskacc, start=False, stop=True)
        P.tt("dve", maskacc, maskacc, msk, ALU.add)
        P.cp("dve", ekf, idx8[:, 0:4])
        P.act(ex4, top8[:, 0:4], AF.Exp, bias=nmx[:, 0:1])
        yield
        P.cp("act", posS, pC[:, NE:2 * NE])
        P.rsum("dve", den, ex4)
        yield
        P.add("dve", lambda e: e.reciprocal(den.ap, den.ap), [den], [den])
        for k in range(4):
            P.stt("dve", tmp32, iotaE, ekf[:, k:k + 1], posS, ALU.is_equal, ALU.mult)
            P.rsum("dve", pk[:, k:k + 1], tmp32)
            yield
        P.ts("dve", val, pk, float(C), ALU.is_lt)
        P.stt("dve", slotf, ekf, float(C), pk, ALU.mult, ALU.add)
        yield
        P.ts("dve", slotf, slotf, -BIG, ALU.add)
        P.ts("dve", ex4, ex4, den[:, 0:1], ALU.mult)
        yield
        P.tt("dve", slotf, slotf, val, ALU.mult)
        P.tt("dve", wkt, ex4, val, ALU.mult)
        yield
        P.ts("dve", slotf, slotf, BIG, ALU.add)
        yield
        P.cp("dve", sI, slotf)
        yield
        for k in range(4):
            def sc(e, k=k, s=s, hb2=hb2):
                return e.indirect_dma_start(
                    out=xg_d.ap, out_offset=bass.IndirectOffsetOnAxis(ap=slotI[:, s, k:k + 1].ap, axis=0),
                    in_=hb2.ap, in_offset=None, bounds_check=P.bc_reg(e, NSLOT - 1), oob_is_err=False)
            P.add("pool", sc, [sI, hb2], [xg_d], dkey="xgsc%d" % q)
        yield

    for s in range(0, NMAIN, 2):
        interleave(tile2a(s), tile2a(s + 1) if s + 1 < NMAIN else None)
    NSB = C // 512
    flags_t = nc.dram_tensor("flags", [1, NSB * NE], I32, kind="Internal")
    flags_d = V(flags_t.ap(), [Buf("flags")])
    P.flags_ap = flags_t.ap()
    P.mm(pb[3][:, 0:NE], onesB, maskacc)
    cntS = sb2("cntS", [128, NE])
    P.cp("act", cntS, pb[3][:, 0:NE])
    flagf = sb2("flagf", [128, NSB * NE])
    for sbk in range(NSB):
        P.ts("dve", flagf[:, sbk * NE:(sbk + 1) * NE], cntS, float(sbk * 512), ALU.is_gt)
    flagt = sb2("flagt", [128, NE])
    P.cp("dve", flagt, flagf[:, 0:NE])
    P.tt("dve", flagf[:, 0:NE - 1], flagt[:, 0:NE - 1], flagt[:, 1:NE], ALU.max)
    flagI = sb2("flagI", [128, NSB * NE], I32)
    P.cp("dve", flagI, flagf)
    P.dma("sp", flags_d, flagI[0:1, :], "flags")
    P.emit()
    s2.close()

    s2 = ExitStack()
    bguT = sb2("bguT", [128, 16, NE])
    s2t = ExitStack()
    bguRows = sb("bguRows", [NE, 2 * D], F32, st=s2t)
    P.dma("sp", bguRows, b_gu, "bguRows")
    for c in range(16):
        P.tr(pb[c % 2][:, 0:NE], bguRows[:, c * 128:(c + 1) * 128], identF[0:NE, 0:NE])
        P.cp("dve", bguT[:, c, :], pb[c % 2][:, 0:NE])
    P.emit()
    s2t.close()
    ones1 = sb2("ones1", [1, 128], BF16)
    P.memset("dve", ones1, 1.0)
    wguB = [sb2("wguB%d" % i, [128, 8, 2 * D], BF16) for i in range(2)]
    wdB = [sb2("wdB%d" % i, [128, 8, D], BF16) for i in range(2)]
    bdr = [sb2("bdr%d" % i, [1, D], BF16) for i in range(2)]
    xr = [sb2("xr%d" % i, [128, D], BF16) for i in range(4)]
    xgT = [sb2("xgT%d" % i, [128, 8, 512], BF16) for i in range(2)]
    actT = [sb2("actT%d" % i, [128, 8, 512], BF16) for i in range(2)]
    actK = [[V(a_.ap[:, k_, :], [Buf("actT%d_%d" % (i_, k_))]) for k_ in range(8)] for i_, a_ in enumerate(actT)]
    gq = [sb2("gq%d" % i, [128, 512]) for i in range(2)]
    uq = [sb2("uq%d" % i, [128, 512]) for i in range(2)]
    sg = sb2("sg0", [128, 512])
    tq = sb2("tq0", [128, 512])
    ysb = [sb2("ysb%d" % i, [128, D], BF16) for i in range(2)]

    w_gu_v = w_gu.re("e (k p) n -> e p k n", p=128)
    w_down_v = w_down.re("e (k p) n -> e p k n", p=128)

    stg = [sb2("stg%d" % i, [128, 2048]) for i in range(2)]

    def load_expert(e, slot):
        for k in range(0, 4, 2):
            P.dma("pool", wguB[slot][:, k:k + 2, :], w_gu_v[e, :, k:k + 2, :], "wgu%d" % slot)
        P.dma("pool", bdr[slot], b_down[e:e + 1, :], "bdr%d" % slot)

    def staged_chunks(e, slot):
        ch = []
        for k in range(4, 8):
            ch.append((w_gu_v[e, :, k, :], wguB[slot][:, k, :], False))
        for k in range(0, 8, 2):
            ch.append((w_down_v[e, :, k:k + 2, :], wdB[slot][:, k:k + 2, :], True))
        return ch

    def stream_steps(e, slot):
        ch = staged_chunks(e, slot)
        n = len(ch)
        acts = []
        for c in range(n + 2):
            def act_(c=c):
                if 0 <= c - 2 < n:
                    src, dst, two = ch[c - 2]
                    i = (c - 2) % 2
                    sv = stg[i].re("p (a n) -> p a n", a=2) if two else stg[i]
                    P.cp("dve", dst, sv)
                if c < n:
                    src, dst, two = ch[c]
                    i = c % 2
                    sv = stg[i].re("p (a n) -> p a n", a=2) if two else stg[i]
                    P.dma("act", sv, src, "stg%d" % i)
            acts.append(act_)
        return acts

    load_expert(0, 0)
    for a_ in stream_steps(0, 0):
        a_()
    nblk = [0]
    nsb = 0

    def row_loads(e, sbk):
        r0_ = e * C + sbk * 512
        rows = []
        for j in range(4):
            xrow = xr[nblk[0] % 4]
            P.dma("sp", xrow, xg_d[r0_ + j * 128:r0_ + (j + 1) * 128, :], "xr%d" % (nblk[0] % 4))
            nblk[0] += 1
            rows.append(xrow)
        return rows

    def row_T(rows, j, xT_):
        for k in range(8):
            P.tr(ptp[:, k * 128:(k + 1) * 128], rows[j][:, k * 128:(k + 1) * 128], identB)
        P.cp("act" if j % 2 == 0 else "dve", xT_[:, :, j * 128:(j + 1) * 128], ptp.re("p (k t) -> p k t", k=8))

    P.cond = (0,) if use_skip else None
    rows0 = row_loads(0, 0)
    for j in range(4):
        row_T(rows0, j, xgT[0])
    P.cond = None
    for e in range(n_exp):
        slot = e % 2
        bsteps = []
        if e + 1 < n_exp:
            load_expert(e + 1, 1 - slot)
            bsteps = stream_steps(e + 1, 1 - slot)
        wg = wguB[slot]
        wd = wdB[slot]
        xT = xgT[e % 2]
        for sbk in range(C // 512):
            if use_skip:
                P.cond = (e,) if sbk == 0 else tuple(k_ * NE + e for k_ in range(1, sbk + 1))
            else:
                P.cond = None
            aT = actT[nsb % 2]
            aK = actK[nsb % 2]
            nsb += 1
            r0 = e * C + sbk * 512
            nxt = None
            if sbk == 0:
                if e + 1 < n_exp:
                    nxt = row_loads(e + 1, 0)
            else:
                rws = row_loads(e, sbk)
                for j in range(4):
                    row_T(rws, j, xT)
            for fc in range(8):
                pg_ = pb[fc % 2]
                pu_ = pb[2 + fc % 2]
                for k in range(8):
                    P.mm(pg_, wg[:, k, fc * 128:(fc + 1) * 128], xT[:, k, :], start=(k == 0), stop=(k == 7))
                for k in range(8):
                    P.mm(pu_, wg[:, k, D + fc * 128:D + (fc + 1) * 128], xT[:, k, :], start=(k == 0), stop=(k == 7))
                i2 = fc % 2
                P.ts("dve", gq[i2], pg_, bguT[:, fc, e:e + 1], ALU.add, 7.0, ALU.min)
                P.act(sg, gq[i2], AF.Sigmoid, scale=1.702)
                P.ts("dve", uq[i2], pu_, bguT[:, 8 + fc, e:e + 1], ALU.add, 7.0, ALU.min)
                P.ts("dve", uq[i2], uq[i2], -7.0, ALU.max, 1.0, ALU.add)
                P.tt("dve" if fc == 7 else "pool", tq, gq[i2], sg, ALU.mult)
                P.tt("dve" if fc == 7 else "pool", aK[fc], tq, uq[i2], ALU.mult)
                if bsteps:
                    P.always = True
                    bsteps.pop(0)()
                    P.always = False
            for j in range(4):
                if nxt is not None:
                    row_T(nxt, j, xgT[(e + 1) % 2])
                yb_ = ysb[j % 2]
                for half in range(2):
                    py = pb[4 + half]
                    for k in range(8):
                        P.mm(py, aK[k][:, j * 128:(j + 1) * 128], wd[:, k, half * 512:(half + 1) * 512],
                             start=(k == 0), stop=False)
                    P.mm(py, ones1, bdr[slot][:, half * 512:(half + 1) * 512], start=False, stop=True)
                    P.cp("act", yb_[:, half * 512:(half + 1) * 512], py)
                P.dma("sp", yy_d[r0 + j * 128:r0 + (j + 1) * 128, :], yb_, "yst%d" % (j % 2))
            P.cond = None
        while bsteps:
            bsteps.pop(0)()
    P.emit()
    s2.close()

    s2 = ExitStack()
    yg = [[sb2("yg%d_%d" % (i, k), [128, D], BF16) for k in range(4)] for i in range(2)]
    for i in range(2):
        for k in range(4):
            P.memset("dve" if k % 2 == 0 else "pool", yg[i][k], 0.0)
    g2R = sb2("g2R", [128, D])
    P.dma("sp", g2R, mod2_d[:, 2 * D:3 * D], "g2R")
    nfR = sb2("nfR", [128, D])
    P.dma("sp", nfR, bcast_rows(norm_f_w, D), "nfR")
    x2 = [sb2("x2c_%d" % i, [128, D]) for i in range(2)]
    acc_ = [sb2("acc%d" % i, [128, D]) for i in range(2)]
    xo2_ = [sb2("xo2_%d" % i, [128, D]) for i in range(2)]
    junk2_ = [sb2("junk2c%d" % i, [128, D]) for i in range(2)]
    ssq2_ = [sb2("ssq2c%d" % i, [128, 1]) for i in range(2)]
    rstd2_ = [sb2("rstd2c%d" % i, [128, 1]) for i in range(2)]

    def tile2c(s):
        q = s % 2
        xs, ygs, acc, xo, junk2, ssq2, rstd2 = x2[q], yg[q], acc_[q], xo2_[q], junk2_[q], ssq2_[q], rstd2_[q]
        sI, wkt = slotI_t[s], wk_t[s]
        P.dma("sp", xs, x1_d[s * 128:(s + 1) * 128, :], "x2c_%d" % q)
        for k in range(4):
            def ga(e, k=k, s=s, ygs=ygs):
                return e.indirect_dma_start(
                    out=ygs[k].ap, out_offset=None, in_=yy_d.ap,
                    in_offset=bass.IndirectOffsetOnAxis(ap=slotI[:, s, k:k + 1].ap, axis=0),
                    bounds_check=P.bc_reg(e, NSLOT - 1), oob_is_err=False)
            P.add("pool", ga, [sI, yy_d], [ygs[k]], dkey="yg%d_%d" % (q, k))
        yield
        P.ts("dve", acc, ygs[0], wkt[:, 0:1], ALU.mult)
        yield
        for k in range(1, 4):
            P.stt("dve", acc, ygs[k], wkt[:, k:k + 1], acc, ALU.mult, ALU.add)
            yield
        P.tt("dve", xo, acc, g2R, ALU.mult)
        yield
        P.tt("dve", xo, xo, xs, ALU.add)
        P.memset("dve", ssq2, 0.0)
        yield
        P.act(junk2, xo, AF.Square, accum=ssq2)
        yield
        P.rsq(rstd2, ssq2, 1.0 / D)
        yield
        P.stt("dve", xo, xo, rstd2, nfR, ALU.mult, ALU.mult)
        yield
        P.dma("sp", out_d[s * 128:(s + 1) * 128, :], xo, "out%d" % q)
        yield

    for s in range(0, NMAIN, 2):
        interleave(tile2c(s), tile2c(s + 1) if s + 1 < NMAIN else None)
    P.emit()
    P.final_wait()
    s2.close()
    s2p.close()
    es.close()
    return nc


def make_consts():
    c = np.zeros((128, 512 + NE), np.float32)
    c[:, 512:] = np.arange(NE, dtype=np.float32)[None, :]
    i = np.arange(128)
    c[:, 0:128] = np.eye(128, dtype=np.float32)
    c[:, 128:256] = (i[None, :] >= i[:, None])
    c[:, 256:384] = (i[None, :] <= i[:, None])
    c[:, 384:512] = (i[None, :] < i[:, None])
    return c


def core_inputs(inp, b, half, npre, nmain):
    x = inp["x"]
    L = 0
    f = np.ascontiguousarray
    if half == 0:
        xs = np.concatenate([x[b, :npre * 128], x[b, :nmain * 128]], axis=0)
        flag = 0.0
    else:
        xs = x[b, :(npre + nmain) * 128]
        flag = 1.0
    m = {
        "x_seq": f(xs),
        "flag": np.full((128, 1), flag, np.float32),
        "c_row": f(inp["c"][b].reshape(8, 128)),
        "consts": make_consts(),
        "w_ada": f(inp["w_ada"][L]),
        "b_ada": f(inp["b_ada"][L].reshape(1, -1)),
        "norm1_w": f(inp["norm1_w"][L].reshape(1, -1)),
        "w_in": f(inp["w_in"][L]),
        "gm_vnorm_w": f(inp["gm_vnorm_w"][L].reshape(1, -1)),
        "gm_w_spatial": f(inp["gm_w_spatial"][L]),
        "gm_b_spatial": f(inp["gm_b_spatial"][L]),
        "gdn_conv_w": f(inp["gdn_conv_w"][L]),
        "gdn_a_log": f(inp["gdn_a_log"][L].reshape(1, -1)),
        "gdn_dt_bias": f(inp["gdn_dt_bias"][L].reshape(1, -1)),
        "gdn_onorm_w": f(inp["gdn_onorm_w"][L].reshape(1, -1)),
        "w_out": f(inp["w_out"][L]),
        "norm2_w": f(inp["norm2_w"][L].reshape(1, -1)),
        "w_router": f(inp["w_router"][L]),
        "b_router": f(inp["b_router"][L].reshape(1, -1)),
        "w_gu": f(inp["w_gu"][L]),
        "b_gu": f(inp["b_gu"][L]),
        "w_down": f(inp["w_down"][L]),
        "b_down": f(inp["b_down"][L]),
        "norm_f_w": f(inp["norm_f_w"].reshape(1, -1)),
    }
    return m


def kernel(**inputs):
    inp = {k: np.asarray(v, dtype=np.float32) for k, v in inputs.items()}
    B, S, _ = inp["x"].shape
    nh = S // 2 // 128
    nc = build(nh, nh)
    in_maps = []
    for c in range(8):
        in_maps.append(core_inputs(inp, c // 2, c % 2, nh, nh))
    res = run_bass_kernel_spmd(nc, in_maps, core_ids=list(range(8)))
    out = np.empty((B, S, D), np.float32)
    for c in range(8):
        b, half = c // 2, c % 2
        out[b, half * (S // 2):(half + 1) * (S // 2)] = res.results[c]["out"]
    return out
```

```python
from contextlib import ExitStack
import numpy as np
import concourse.bass as bass
import concourse.mybir as mybir
from concourse.bass_utils import run_bass_kernel_spmd

F32 = mybir.dt.float32
BF16 = mybir.dt.bfloat16
AF = mybir.ActivationFunctionType
ALU = mybir.AluOpType
AX = mybir.AxisListType

D = 1024
NCOLS = 3080
NE = 32
EPS = 1e-6


class Buf:
    __slots__ = ("name", "w", "r", "excl")

    def __init__(self, name, excl=False):
        self.name = name
        self.w = {}
        self.r = {}
        self.excl = excl


class V:
    __slots__ = ("ap", "bufs")

    def __init__(self, ap, bufs):
        self.ap = ap
        self.bufs = bufs

    def __getitem__(self, key):
        return V(self.ap[key], self.bufs)

    def re(self, s, **kw):
        return V(self.ap.rearrange(s, **kw), self.bufs)

    def bc(self, shape):
        return V(self.ap.to_broadcast(shape), self.bufs)


class Op:
    __slots__ = ("eng", "fn", "deps", "idx", "inc", "val", "dkey", "cond", "dn", "always")


COMPUTE = ("pe", "act", "dve", "pool")


class Prog:
    def __init__(self, nc):
        self.nc = nc
        self.ops = {e: [] for e in COMPUTE + ("sp",)}
        self.emitted = {e: 0 for e in COMPUTE + ("sp",)}
        self.dcount = {}
        self.sems = {}
        self.es = ExitStack()
        self.seen = {e: {} for e in COMPUTE + ("sp",)}
        self.ninc = {e: 0 for e in COMPUTE}
        self.barrier = None
        self.regs = {}
        self.cond = None
        self.always = False
        self.dry = False
        self.flags_ap = None

    def bc_reg(self, e, val):
        if val not in self.regs:
            reg = e.alloc_register("bcr%d" % val)
            e.reg_mov(reg, val)
            self.regs[val] = reg
        return self.regs[val]

    def sem(self, name):
        if name not in self.sems:
            self.sems[name] = self.es.enter_context(self.nc.semaphore("s_" + name.replace(":", "_")))
        return self.sems[name]

    def add(self, eng, fn, reads, writes, dkey=None):
        if self.dry:
            return None
        op = Op()
        op.eng = eng
        op.fn = fn
        op.inc = False
        op.val = None
        op.dkey = dkey
        op.cond = self.cond
        op.always = self.always
        op.dn = None
        op.idx = len(self.ops[eng])
        deps = {}

        def need(src, idx):
            if src == "pe" and eng == "pe" and dkey is None:
                return
            if deps.get(src, -1) < idx:
                deps[src] = idx

        rb = [b for v in reads if isinstance(v, V) for b in v.bufs]
        wb = [b for v in writes if isinstance(v, V) for b in v.bufs]
        for b in rb:
            for s, i in b.w.items():
                need(s, i)
            if b.excl:
                for s, i in b.r.items():
                    if s != eng:
                        need(s, i)
        for b in wb:
            for s, i in b.w.items():
                need(s, i)
            for s, i in b.r.items():
                need(s, i)
        op.deps = deps
        if dkey is not None:
            self.dcount[dkey] = self.dcount.get(dkey, 0) + 1
            op.dn = self.dcount[dkey]
            me = ("D:" + dkey, self.dcount[dkey])
        else:
            me = (eng, op.idx)
        for b in rb:
            b.r[me[0]] = me[1]
        for b in wb:
            b.w[me[0]] = me[1]
        self.ops[eng].append(op)
        return op

    def emit(self):
        nc = self.nc
        for e in self.ops:
            for op in self.ops[e][self.emitted[e]:]:
                for s, i in op.deps.items():
                    if not s.startswith("D:") and i >= self.emitted[s]:
                        self.ops[s][i].inc = True
        for e in COMPUTE:
            if len(self.ops[e]) > self.emitted[e]:
                self.ops[e][-1].inc = True
        for e in COMPUTE:
            for op in self.ops[e][self.emitted[e]:]:
                if op.inc and op.dkey is None:
                    self.ninc[e] += 1
                    op.val = self.ninc[e]
        engs = {"pe": nc.tensor, "act": nc.scalar, "dve": nc.vector, "pool": nc.gpsimd, "sp": nc.sync}
        for k in self.dcount:
            self.sem("D:" + k)
        for e in COMPUTE:
            self.sem(e)
        barrier = self.barrier
        start = dict(self.emitted)

        def run(ename, engine):
            seen = self.seen[ename]
            ops = self.ops[ename][self.emitted[ename]:]
            plan = []
            first = True
            for op in ops:
                waits = {}
                if first and barrier is not None:
                    for s, v in barrier.items():
                        waits[s] = v
                first = False
                for s, i in op.deps.items():
                    if s.startswith("D:"):
                        v = 16 * i
                    else:
                        if i < start[s]:
                            continue
                        v = self.ops[s][i].val
                        assert v is not None, (s, i)
                    if waits.get(s, 0) < v:
                        waits[s] = v
                wl = []
                for s, v in waits.items():
                    if seen.get(s, 0) >= v:
                        continue
                    seen[s] = v
                    wl.append((s, v))
                plan.append((op, wl))

            def emit_real(group):
                for op, wl in group:
                    for s, v in wl:
                        engine.wait_ge(self.sem(s), v)
                    ins = op.fn(engine)
                    if op.dkey is not None:
                        ins.then_inc(self.sem("D:" + op.dkey), 16)
                    elif op.inc:
                        ins.then_inc(self.sem(ename), 1)

            def emit_skip(group):
                pend_inc = {}
                pend_wait = {}

                def flush():
                    for k, (before, tot) in pend_inc.items():
                        engine.wait_ge(self.sem(k), before)
                        engine.sem_inc(self.sem(k), tot)
                    for s_, v in pend_wait.items():
                        engine.wait_ge(self.sem(s_), v)
                    pend_inc.clear()
                    pend_wait.clear()

                for op, wl in group:
                    if op.always:
                        flush()
                        emit_real([(op, wl)])
                        continue
                    if op.dkey is not None:
                        k = "D:" + op.dkey
                        if k not in pend_inc:
                            pend_inc[k] = [16 * (op.dn - 1), 0]
                        pend_inc[k][1] += 16
                    elif op.inc:
                        if ename not in pend_inc:
                            pend_inc[ename] = [op.val - 1, 0]
                        pend_inc[ename][1] += 1
                    for s_, v in wl:
                        if pend_wait.get(s_, 0) < v:
                            pend_wait[s_] = v
                flush()

            def tag_at(op, depth):
                c = op.cond
                if c is None or len(c) <= depth:
                    return None
                return c[depth]

            def emit_level(items, depth):
                i = 0
                while i < len(items):
                    tag = tag_at(items[i][0], depth)
                    j = i
                    while j < len(items) and tag_at(items[j][0], depth) == tag:
                        j += 1
                    group = items[i:j]
                    if tag is None:
                        emit_real(group)
                    else:
                        key = "cr_" + ename
                        if key not in self.regs:
                            self.regs[key] = engine.alloc_register(key)
                        reg = self.regs[key]
                        engine.reg_load(reg, self.flags_ap[0:1, tag:tag + 1])
                        with engine.If_eq(reg, 0):
                            emit_skip(group)
                        with engine.Else():
                            emit_level(group, depth + 1)
                    i = j

            emit_level(plan, 0)

        with nc.Block() as block:
            @block.tensor
            def _(eng):
                run("pe", eng)

            @block.scalar
            def _(eng):
                run("act", eng)

            @block.vector
            def _(eng):
                run("dve", eng)

            @block.gpsimd
            def _(eng):
                run("pool", eng)

            @block.sync
            def _(eng):
                run("sp", eng)
        for e in self.ops:
            self.emitted[e] = len(self.ops[e])
        b = {}
        for e in COMPUTE:
            if self.ninc[e]:
                b[e] = self.ninc[e]
        for k, c in self.dcount.items():
            b["D:" + k] = 16 * c
        self.barrier = b

    def final_wait(self):
        nc = self.nc
        b = self.barrier
        with nc.Block() as block:
            @block.sync
            def _(eng):
                for s, v in b.items():
                    eng.wait_ge(self.sem(s), v)
        self.es.close()

    def mm(self, out, lhsT, rhs, start=True, stop=True):
        rd = [lhsT, rhs] + ([] if start else [out])
        self.add("pe", lambda e: e.matmul(out.ap, lhsT.ap, rhs.ap, start=start, stop=stop), rd, [out])

    def tr(self, out, in_, ident):
        self.add("pe", lambda e: e.transpose(out.ap, in_.ap, ident.ap), [in_, ident], [out])

    def act(self, out, in_, func, bias=None, scale=1.0, accum=None, eng="act"):
        rd = [in_]
        kw = {}
        if bias is not None:
            kw["bias"] = bias.ap if isinstance(bias, V) else bias
            rd.append(bias)
        if isinstance(scale, V):
            kw["scale"] = scale.ap
            rd.append(scale)
        else:
            kw["scale"] = scale
        wr = [out]
        if accum is not None:
            kw["accum_out"] = accum.ap
            wr.append(accum)
        self.add(eng, lambda e: e.activation(out.ap, in_.ap, func, **kw), rd, wr)

    def tt(self, eng, out, a, b, op):
        self.add(eng, lambda e: e.tensor_tensor(out.ap, a.ap, b.ap, op), [a, b], [out])

    def ts(self, eng, out, a, s1, op0, s2=None, op1=None):
        rd = [a]
        x1 = s1.ap if isinstance(s1, V) else s1
        x2 = s2.ap if isinstance(s2, V) else s2
        if isinstance(s1, V):
            rd.append(s1)
        if isinstance(s2, V):
            rd.append(s2)
        if op1 is None:
            self.add(eng, lambda e: e.tensor_scalar(out.ap, a.ap, x1, None, op0), rd, [out])
        else:
            self.add(eng, lambda e: e.tensor_scalar(out.ap, a.ap, x1, x2, op0, op1), rd, [out])

    def stt(self, eng, out, in0, scalar, in1, op0, op1):
        rd = [in0, in1]
        sc = scalar.ap if isinstance(scalar, V) else scalar
        if isinstance(scalar, V):
            rd.append(scalar)
        self.add(eng, lambda e: e.scalar_tensor_tensor(out.ap, in0.ap, sc, in1.ap, op0, op1), rd, [out])

    def rsq(self, out, in_, scale=1.0):
        self.act(out, in_, AF.Ln, bias=EPS, scale=scale)
        self.act(out, out, AF.Exp, scale=-0.5)

    def cp(self, eng, out, in_):
        if eng == "act":
            self.add(eng, lambda e: e.copy(out.ap, in_.ap), [in_], [out])
        else:
            self.add(eng, lambda e: e.tensor_copy(out.ap, in_.ap), [in_], [out])

    def memset(self, eng, out, val):
        self.add(eng, lambda e: e.memset(out.ap, val), [], [out])

    def rsum(self, eng, out, in_):
        self.add(eng, lambda e: e.reduce_sum(out.ap, in_.ap, AX.X), [in_], [out])

    def dma(self, eng, out, in_, key, slow=False):
        if slow:
            self.add(eng, lambda e: e.dma_start(out.ap, in_.ap, allow_slow_non_contiguous=True), [in_], [out], dkey=key)
        else:
            self.add(eng, lambda e: e.dma_start(out.ap, in_.ap), [in_], [out], dkey=key)


def build(NPRE, NMAIN, debug=False, n_exp=NE, cap=2048, use_skip=True, PIPE=True):
    nc = bass.Bass("TRN2", target_bir_lowering=False)
    P = Prog(nc)
    NT = NPRE + NMAIN
    NTOK = NMAIN * 128

    def din(name, shape):
        t = nc.dram_tensor(name, list(shape), F32, kind="ExternalInput")
        return V(t.ap(), [Buf(name)])

    x_seq = din("x_seq", [NT * 128, D])
    flag_d = din("flag", [128, 1])
    c_row = din("c_row", [8, 128])
    consts_all = din("consts", [128, 512 + NE])
    consts = consts_all[:, 0:512]
    consts2 = consts_all[:, 512:512 + NE]
    w_ada = din("w_ada", [D, 6 * D])
    b_ada = din("b_ada", [1, 6 * D])
    norm1_w = din("norm1_w", [1, D])
    w_in = din("w_in", [D, NCOLS])
    gm_vnorm_w = din("gm_vnorm_w", [1, 512])
    gm_w_spatial = din("gm_w_spatial", [4, 128, 128])
    gm_b_spatial = din("gm_b_spatial", [4, 128])
    gdn_conv_w = din("gdn_conv_w", [4, 1536])
    gdn_a_log = din("gdn_a_log", [1, 4])
    gdn_dt_bias = din("gdn_dt_bias", [1, 4])
    gdn_onorm_w = din("gdn_onorm_w", [1, 128])
    w_out = din("w_out", [D, D])
    norm2_w = din("norm2_w", [1, D])
    w_router = din("w_router", [D, NE])
    b_router = din("b_router", [1, NE])
    w_gu = din("w_gu", [NE, D, 2 * D])
    b_gu = din("b_gu", [NE, 2 * D])
    w_down = din("w_down", [NE, D, D])
    b_down = din("b_down", [NE, D])
    norm_f_w = din("norm_f_w", [1, D])
    out_t = nc.dram_tensor("out", [NTOK, D], F32, kind="ExternalOutput")
    out_d = V(out_t.ap(), [Buf("out")])
    x1_t = nc.dram_tensor("x1s", [NTOK, D], F32, kind="Internal")
    x1_d = V(x1_t.ap(), [Buf("x1s")])
    dbg = {}
    if debug:
        for nm, shp in debug.items():
            t = nc.dram_tensor(nm, list(shp), F32, kind="ExternalOutput")
            dbg[nm] = V(t.ap(), [Buf(nm)])

    es = ExitStack()

    def sb(name, shape, dt=F32, st=None):
        t = (st or es).enter_context(nc.sbuf_tensor(name, list(shape), dt))
        return V(t[:], [Buf(name)])

    def ps(name, shape, dt=F32, st=None):
        t = (st or es).enter_context(nc.psum_tensor(name, list(shape), dt))
        return V(t[:], [Buf(name, excl=True)])

    def bcast_rows(src, n):
        return V(src.ap[0:1, :].to_broadcast([128, n]), src.bufs)

    cst = sb("cst", [128, 512])
    mod1 = sb("mod1", [128, 3 * D])
    identB = sb("identB", [128, 128], BF16)
    onesB = sb("onesB", [128, 128], BF16)
    flagT = sb("flagT", [128, 1])
    identF = cst[:, 0:128]
    triu = cst[:, 128:256]
    tril = cst[:, 256:384]
    stril = cst[:, 384:512]
    sh1 = mod1[:, 0:D]
    A1 = mod1[:, D:2 * D]
    g1R = mod1[:, 2 * D:3 * D]
    mod2_t = nc.dram_tensor("mod2s", [128, 3 * D], F32, kind="Internal")
    mod2_d = V(mod2_t.ap(), [Buf("mod2s")])

    P.dma("sp", cst, consts, "cst")
    P.dma("sp", flagT, flag_d, "flag")
    P.cp("dve", identB, identF)
    P.memset("dve", onesB, 1.0)

    pb = [ps("pb%d" % i, [128, 512]) for i in range(7)]
    ptp = ps("ptp", [128, 1024], BF16)

    s1 = ExitStack()

    def sb1(name, shape, dt=F32):
        return sb(name, shape, dt, st=s1)

    w_inB = sb1("w_inB", [128, 8, NCOLS], BF16)
    w_in_v = w_in.re("(k p) n -> p k n", p=128)
    for k in range(8):
        P.dma("pool", w_inB[:, k, :], w_in_v[:, k, :], "w_inB")
    w_outB = sb1("w_outB", [128, 8, D], BF16)
    w_out_v = w_out.re("(k p) n -> p k n", p=128)
    for k in range(0, 8, 4):
        P.dma("pool", w_outB[:, k:k + 4, :], w_out_v[:, k:k + 4, :], "w_outB")
    s0 = ExitStack()

    def sb0(name, shape, dt=F32):
        return sb(name, shape, dt, st=s0)

    modR = sb0("modR", [128, 6 * D])
    A1t = modR[:, D:2 * D]
    A2t = modR[:, 4 * D:5 * D]
    cT = sb0("cT", [128, 8])
    cact = sb0("cact", [128, 8])
    cactB = sb0("cactB", [128, 8, 128])
    P.dma("sp", cT, c_row.re("k p -> p k"), "cT", slow=True)
    P.act(cact, cT, AF.Silu)
    for k in range(8):
        P.cp("dve", cactB[:, k, :], cact[:, k:k + 1].bc([128, 128]))
    P.dma("sp", modR, bcast_rows(b_ada, 6 * D), "modR")
    wst = [sb0("wst%d" % i, [128, 8, 512]) for i in range(2)]
    w_ada_v = w_ada.re("(k p) n -> p k n", p=128)
    for cb in range(12):
        st = wst[cb % 2]
        P.dma("sp", st, w_ada_v[:, :, cb * 512:(cb + 1) * 512], "wst%d" % (cb % 2))
        pm = pb[cb % 2]
        for k in range(8):
            P.mm(pm, cactB[:, k, :], st[:, k, :], start=(k == 0), stop=(k == 7))
        P.tt("dve", modR[:, cb * 512:(cb + 1) * 512], modR[:, cb * 512:(cb + 1) * 512], pm, ALU.add)
    nwR = sb0("nwR", [128, D])
    P.dma("sp", nwR, bcast_rows(norm1_w, D), "nwR")
    P.stt("dve", A1t, A1t, 1.0, nwR, ALU.add, ALU.mult)
    nwR2 = sb0("nwR2", [128, D])
    P.dma("sp", nwR2, bcast_rows(norm2_w, D), "nwR2")
    P.stt("dve", A2t, A2t, 1.0, nwR2, ALU.add, ALU.mult)
    P.cp("act", mod1, modR[:, 0:3 * D])
    P.dma("sp", mod2_d, modR[:, 3 * D:6 * D], "mod2s")
    P.emit()
    s0.close()
    import os
    KSTOP = int(os.environ.get("KSTOP", "9"))
    KCUT = int(os.environ.get("KCUT", "99"))
    KSUB = int(os.environ.get("KSUB", "99"))
    if KSTOP == 0:
        P.final_wait()
        return nc
    if KSTOP == 1:
        NT = 0

    wsF = sb1("wsF", [128, 4, 128])
    P.dma("sp", wsF, gm_w_spatial.re("g t s -> t g s"), "wsF")
    for g in range(4):
        P.tt("dve", wsF[:, g, :], wsF[:, g, :], tril, ALU.mult)
    wsT = sb1("wsT", [128, 4, 128], BF16)
    for g in range(4):
        P.tr(pb[2][:, g * 128:(g + 1) * 128], wsF[:, g, :], identF)
    P.cp("dve", wsT.re("p g t -> p (g t)"), pb[2])
    bsp = sb1("bsp", [128, 4])
    P.dma("sp", bsp, gm_b_spatial.re("g t -> t g"), "bsp", slow=True)
    vnwR = sb1("vnwR", [128, 512])
    P.dma("sp", vnwR, bcast_rows(gm_vnorm_w, 512), "vnwR")
    convw = sb1("convw", [128, 4, 12])
    for j in range(4):
        P.dma("sp", convw[:, j, :], gdn_conv_w[j, :].re("(c p) -> p c", p=128), "convw", slow=True)
    alog = sb1("alog", [128, 4])
    dtb = sb1("dtb", [128, 4])
    nexpA = sb1("nexpA", [128, 4])
    P.dma("sp", alog, bcast_rows(gdn_a_log, 4), "alog")
    P.dma("sp", dtb, bcast_rows(gdn_dt_bias, 4), "dtb")
    P.act(nexpA, alog, AF.Exp)
    P.ts("dve", nexpA, nexpA, -1.0, ALU.mult)
    onwR = sb1("onwR", [128, 128])
    P.dma("sp", onwR, bcast_rows(gdn_onorm_w, 128), "onwR")

    xt = [sb1("xt%d" % i, [128, D]) for i in range(2)]
    ssq = sb1("ssq", [128, 1])
    rstd = sb1("rstd", [128, 1])
    hf = sb1("hf", [128, D])
    junk = hf
    hb = sb1("hb", [128, D], BF16)
    hT = sb1("hT", [128, D], BF16)
    uS = sb1("uS", [128, 512])
    gvS = sb1("gvS", [128, 512])
    vss = sb1("vss", [128, 4])
    vr = sb1("vr", [128, 4])
    vn = sb1("vn", [128, 512], BF16)
    ycat = [sb1("ycat%d" % i, [128, D], BF16) for i in range(2)]
    yT = sb1("yT", [128, D], BF16)
    cbuf = sb1("cbuf", [128, 12, 131])
    cvaA = sb1("cvaA", [128, 8, 128])
    cvaB = sb1("cvaB", [128, 4, 128])
    cvbA = sb1("cvbA", [128, 8, 128])
    cvbB = sb1("cvbB", [128, 4, 128])
    qkvS = sb1("qkvS", [128, 12, 128])
    sqn = sb1("sqn", [128, 8, 128], BF16)
    sq5 = cvaA[:, 0:4, :].re("p h e -> p (h e)")
    rn = cvbA
    t1 = cvbB
    Din = cvaA[:, 4:8, :]
    DTin = cvaA[:, 4:8, :]
    Dm = cvaB
    qT = sb1("qT", [128, 4, 128], BF16)
    kT = sb1("kT", [128, 4, 128], BF16)
    vTb = sb1("vTb", [128, 4, 128], BF16)
    abx = sb1("abx", [128, 4])
    aex = sb1("aex", [128, 4])
    asp = sb1("asp", [128, 4])
    gS = sb1("gS", [128, 4])
    beta = sb1("beta", [128, 4])
    nbeta = sb1("nbeta", [128, 4])
    gcS = sb1("gcS", [128, 4])
    egct = sb1("egct", [128, 4])
    bg = sb1("bg", [128, 4])
    dlin = sb1("dlin", [128, 4])
    dl = sb1("dl", [128, 4])
    gB = sb1("gB", [128, 4, 128])
    egcB = sb1("egcB", [128, 4, 128])
    DTm = sb1("DTm", [128, 4, 128])
    N0 = [sb1("N0_%d" % i, [128, 4, 128]) for i in range(2)]
    NT0 = [sb1("NT0_%d" % i, [128, 4, 128]) for i in range(2)]
    R0 = [sb1("R0_%d" % i, [128, 4, 128]) for i in range(2)]
    vb = [sb1("vb%d" % i, [128, 4, 128], BF16) for i in range(2)]
    kbg = [sb1("kbg%d" % i, [128, 4, 128], BF16) for i in range(2)]
    kdec = [sb1("kdec%d" % i, [128, 4, 128], BF16) for i in range(2)]
    qgT = [sb1("qgT%d" % i, [128, 4, 128], BF16) for i in range(2)]
    qkTb = [sb1("qkTb%d" % i, [128, 4, 128], BF16) for i in range(2)]
    egl = [sb1("egl%d" % i, [128, 4]) for i in range(2)]
    gz = [sb1("gz%d" % i, [128, 4, 128]) for i in range(2)]
    Pm = sb1("Pm", [128, 4, 128])
    PTm = sb1("PTm", [128, 4, 128])
    RT = sb1("RT", [128, 4, 128])
    TTb = sb1("TTb", [128, 4, 128], BF16)
    uU = sb1("uU", [128, 4, 128])
    osq = uU
    wTb = sb1("wTb", [128, 4, 128], BF16)
    vnew = sb1("vnew", [128, 4, 128], BF16)
    Sf = sb1("Sf", [128, 4, 128])
    Sb = sb1("Sb", [128, 4, 128], BF16)
    szS = sb1("szS", [128, 512])
    oss = sb1("oss", [128, 4])
    orr = sb1("orr", [128, 4])
    x1t = sb1("x1t0", [128, D])

    P.memset("dve", cbuf, 0.0)
    P.memset("dve", Sf, 0.0)
    P.memset("dve", Sb, 0.0)

    def f3(v):
        return v.re("p (h e) -> p h e", h=4)

    def f2(v):
        return v.re("p h e -> p (h e)")

    def gmlp(t):
        p = t % 2
        sq5g = szS
        yield
        yield
        yield

        def proj1(c0):
            for k in range(8):
                P.mm(pb[1], hT[:, k * 128:(k + 1) * 128], w_inB[:, k, c0:c0 + 512], start=(k == 0), stop=(k == 7))

        proj1(512)
        yield
        P.act(gvS, pb[1], AF.Gelu)
        yield
        proj1(0)
        P.tt("pool", sq5g, gvS, gvS, ALU.mult)
        yield
        P.act(uS, pb[1], AF.Gelu)
        P.rsum("dve", vss, f3(sq5g))
        yield
        P.rsq(vr, vss, 1.0 / 128)
        yield
        for g in range(4):
            P.stt("dve", vn[:, g * 128:(g + 1) * 128], gvS[:, g * 128:(g + 1) * 128], vr[:, g:g + 1],
                  vnwR[:, g * 128:(g + 1) * 128], ALU.mult, ALU.mult)
            if g % 2 == 1:
                yield
        for g in range(4):
            P.mm(pb[1][:, g * 128:(g + 1) * 128], wsT[:, g, :], vn[:, g * 128:(g + 1) * 128])
        yield
        for g in range(4):
            P.stt("dve", ycat[p][:, g * 128:(g + 1) * 128], pb[1][:, g * 128:(g + 1) * 128], bsp[:, g:g + 1],
                  uS[:, g * 128:(g + 1) * 128], ALU.add, ALU.mult)
            if g % 2 == 1:
                yield
        proj1(2560)
        yield
        P.act(szS, pb[1], AF.Silu)
        yield
        P.tt("pool", gz[p], f3(szS), onwR[:, None, :].bc([128, 4, 128]), ALU.mult)
        yield

    def stage1(t):
        main = t >= NPRE
        p = t % 2
        xs = xt[p]
        P.dma("sp", xs, x_seq[t * 128:(t + 1) * 128, :], "xt%d" % p)
        P.memset("dve", ssq, 0.0)
        P.act(junk, xs, AF.Square, accum=ssq)
        P.rsq(rstd, ssq, 1.0 / D)
        yield
        P.stt("dve", hf, xs, rstd, A1, ALU.mult, ALU.mult)
        P.tt("dve", hb, hf, sh1, ALU.add)
        yield
        for k in range(8):
            P.tr(ptp[:, k * 128:(k + 1) * 128], hb[:, k * 128:(k + 1) * 128], identB)
        P.cp("act", hT, ptp)
        yield

        def proj_tok(pso, c0, n):
            for k in range(8):
                P.mm(pso, hT[:, k * 128:(k + 1) * 128], w_inB[:, k, c0:c0 + n], start=(k == 0), stop=(k == 7))

        g0 = 0 if (main or t == NPRE - 1) else 1
        for grp in range(g0, 3):
            pq = (pb[0], pb[4], pb[5])[grp]
            for c in range(4):
                col = 1024 + (grp * 4 + c) * 128
                for k in range(8):
                    P.mm(pq[:, c * 128:(c + 1) * 128], w_inB[:, k, col:col + 128], hT[:, k * 128:(k + 1) * 128],
                         start=(k == 0), stop=(k == 7))
            P.cp("act", cbuf[:, grp * 4:(grp + 1) * 4, 3:131], f3(pq))
            yield
        for k in range(8):
            P.mm(pb[4][:, 0:8], hT[:, k * 128:(k + 1) * 128], w_inB[:, k, 3072:3080], start=(k == 0), stop=(k == 7))
        c0 = 0 if main else 4

        def cwA(j):
            return convw[:, j, c0:8, None].bc([128, 8 - c0, 128])

        def cwB(j):
            return convw[:, j, 8:12, None].bc([128, 4, 128])

        P.tt("dve", cvaA[:, c0:8, :], cbuf[:, c0:8, 0:128], cwA(0), ALU.mult)
        P.tt("pool", cvaB, cbuf[:, 8:12, 0:128], cwB(0), ALU.mult)
        P.tt("dve", cvbA[:, c0:8, :], cbuf[:, c0:8, 1:129], cwA(1), ALU.mult)
        P.tt("pool", cvbB, cbuf[:, 8:12, 1:129], cwB(1), ALU.mult)
        yield
        P.tt("dve", cvaA[:, c0:8, :], cvaA[:, c0:8, :], cvbA[:, c0:8, :], ALU.add)
        P.tt("pool", cvaB, cvaB, cvbB, ALU.add)
        P.tt("dve", cvbA[:, c0:8, :], cbuf[:, c0:8, 2:130], cwA(2), ALU.mult)
        P.tt("pool", cvbB, cbuf[:, 8:12, 2:130], cwB(2), ALU.mult)
        yield
        P.tt("dve", cvaA[:, c0:8, :], cvaA[:, c0:8, :], cvbA[:, c0:8, :], ALU.add)
        P.tt("pool", cvaB, cvaB, cvbB, ALU.add)
        P.tt("dve", cvbA[:, c0:8, :], cbuf[:, c0:8, 3:131], cwA(3), ALU.mult)
        P.tt("pool", cvbB, cbuf[:, 8:12, 3:131], cwB(3), ALU.mult)
        yield
        P.tt("dve", cvaA[:, c0:8, :], cvaA[:, c0:8, :], cvbA[:, c0:8, :], ALU.add)
        P.tt("pool", cvaB, cvaB, cvbB, ALU.add)
        P.cp("act", cbuf[:, :, 0:3], cbuf[:, :, 128:131])
        if t == NPRE - 1:
            P.ts("dve", cbuf[:, :, 0:3], cbuf[:, :, 0:3], flagT[:, 0:1], ALU.mult)
        P.act(qkvS[:, c0:8, :], cvaA[:, c0:8, :], AF.Silu)
        P.act(qkvS[:, 8:12, :], cvaB, AF.Silu)
        yield
        P.act(beta, pb[4][:, 4:8], AF.Sigmoid)
        P.tt("dve", abx, pb[4][:, 0:4], dtb, ALU.add)
        P.act(aex, abx, AF.Exp)
        P.act(asp, aex, AF.Ln, bias=1.0)
        yield
        P.tt("dve", sqn[:, c0:8, :], qkvS[:, c0:8, :], qkvS[:, c0:8, :], ALU.mult)
        P.tt("dve", gS, asp, nexpA, ALU.mult)
        P.ts("dve", nbeta, beta, -1.0, ALU.mult)
        yield
        if main:
            P.mm(pb[0], onesB, f2(sqn[:, 0:4, :]))
        P.mm(pb[5], onesB, f2(sqn[:, 4:8, :]))
        P.mm(pb[4][:, 8:12], triu, gS)
        P.cp("act", gB, gS[:, :, None].bc([128, 4, 128]))
        yield
        if main:
            P.rsq(f2(rn[:, 0:4, :]), pb[0])
        P.rsq(f2(rn[:, 4:8, :]), pb[5])
        P.cp("dve", gcS, pb[4][:, 8:12])
        for h in range(4):
            P.mm(pb[5][:, h * 128:(h + 1) * 128], gB[:, h, :], triu)
        yield
        if main:
            P.stt("dve", qT, qkvS[:, 0:4, :], float(128 ** -0.5), rn[:, 0:4, :], ALU.mult, ALU.mult)
        P.tt("dve", kT, qkvS[:, 4:8, :], rn[:, 4:8, :], ALU.mult)
        P.cp("act", vTb, qkvS[:, 8:12, :])
        P.act(f2(egcB), pb[5], AF.Exp)
        P.act(egct, gcS, AF.Exp)
        yield
        for h in range(4):
            P.tr(ptp[:, h * 128:(h + 1) * 128], kT[:, h, :], identB)
        for h in range(4):
            P.tr(ptp[:, 512 + h * 128:512 + (h + 1) * 128], vTb[:, h, :], identB)
        P.tt("dve", bg, beta, egct, ALU.mult)
        P.tt("dve", dlin, f3(pb[5])[:, :, 127], gcS, ALU.subtract)
        P.act(dl, dlin, AF.Exp)
        yield
        for h in range(4):
            P.mm(pb[4][:, h * 128:(h + 1) * 128], kT[:, h, :], kT[:, h, :])
        for h in range(4):
            P.ts("dve", Din[:, h, :], pb[5][:, h * 128:(h + 1) * 128], gcS[:, h:h + 1], ALU.subtract, 0.0, ALU.max)
        P.act(Dm, Din, AF.Exp, scale=-1.0)
        P.tt("pool", Dm, Dm, stril[:, None, :].bc([128, 4, 128]), ALU.mult)
        yield
        ktok = f3(ptp[:, 0:512])
        vtok = f3(ptp[:, 512:1024])
        P.tt("dve", kbg[p], ktok, bg[:, :, None].bc([128, 4, 128]), ALU.mult)
        P.tt("dve", kdec[p], ktok, dl[:, :, None].bc([128, 4, 128]), ALU.mult)
        P.tt("dve", vb[p], vtok, beta[:, :, None].bc([128, 4, 128]), ALU.mult)
        P.cp("act", egl[p], egcB[:, :, 127])
        yield
        P.tt("dve", t1, f3(pb[4]), Dm, ALU.mult)
        P.tt("dve", N0[p], t1, nbeta[:, :, None].bc([128, 4, 128]), ALU.mult)
        yield
        if main:
            for h in range(4):
                P.ts("dve", DTin[:, h, :], pb[5][:, h * 128:(h + 1) * 128], gcS[:, h:h + 1], ALU.subtract, 0.0, ALU.min)
            P.act(DTm, DTin, AF.Exp)
        yield
        for h in range(4):
            P.tr(pb[0][:, h * 128:(h + 1) * 128], N0[p][:, h, :], identF)
        if main:
            P.tt("pool", DTm, DTm, triu[:, None, :].bc([128, 4, 128]), ALU.mult)
            for h in range(4):
                P.mm(pb[4][:, h * 128:(h + 1) * 128], kT[:, h, :], qT[:, h, :])
        yield
        P.cp("act", f2(NT0[p]), pb[0])
        P.tt("dve", R0[p], f3(pb[0]), identF[:, None, :].bc([128, 4, 128]), ALU.add)
        if main:
            P.tt("pool", qgT[p], qT, egcB, ALU.mult)
            P.tt("dve", qkTb[p], f3(pb[4]), DTm, ALU.mult)
        yield

    def stage2(t):
        main = t >= NPRE
        p = t % 2
        xs = xt[p]
        for lvl in range(1, 7):
            last = lvl == 6
            Pc = N0[p] if lvl == 1 else Pm
            PTc = NT0[p] if lvl == 1 else PTm
            Rc = R0[p] if lvl == 1 else RT
            for h in range(4):
                P.mm(pb[2][:, h * 128:(h + 1) * 128], PTc[:, h, :], Pc[:, h, :])
            if not last:
                for h in range(4):
                    P.mm(pb[3][:, h * 128:(h + 1) * 128], Pc[:, h, :], PTc[:, h, :])
            P.cp("act", f2(Pm), pb[2])
            if not last:
                P.cp("dve", f2(PTm), pb[3])
            yield
            for h in range(4):
                P.mm(pb[6][:, h * 128:(h + 1) * 128], Pm[:, h, :], Rc[:, h, :])
            P.tt("dve", f2(RT), f2(Rc), pb[6], ALU.add)
            yield
        P.cp("act", TTb, RT)
        yield
        for h in range(4):
            P.mm(pb[2][:, h * 128:(h + 1) * 128], TTb[:, h, :], vb[p][:, h, :])
        for h in range(4):
            P.mm(pb[3][:, h * 128:(h + 1) * 128], kbg[p][:, h, :], TTb[:, h, :])
        yield
        P.cp("act", f2(uU), pb[2])
        P.cp("act", f2(wTb), pb[3])
        yield
        for h in range(4):
            P.mm(pb[6][:, h * 128:(h + 1) * 128], wTb[:, h, :], Sb[:, h, :])
        yield
        P.tt("dve", f2(vnew), f2(uU), pb[6], ALU.subtract)
        yield
        if main:
            for h in range(4):
                P.mm(pb[3][:, h * 128:(h + 1) * 128], qgT[p][:, h, :], Sb[:, h, :], start=True, stop=False)
                P.mm(pb[3][:, h * 128:(h + 1) * 128], qkTb[p][:, h, :], vnew[:, h, :], start=False, stop=True)
        for h in range(4):
            P.mm(pb[2][:, h * 128:(h + 1) * 128], kdec[p][:, h, :], vnew[:, h, :])
        yield
        for h in range(4):
            P.stt("dve", Sf[:, h, :], Sf[:, h, :], egl[p][:, h:h + 1], pb[2][:, h * 128:(h + 1) * 128], ALU.mult, ALU.add)
        if t == NPRE - 1:
            P.ts("dve", f2(Sf), f2(Sf), flagT[:, 0:1], ALU.mult)
        P.cp("act", Sb, Sf)
        yield
        if main:
            P.act(f2(osq), pb[3], AF.Square)
            yield
            P.rsum("dve", oss, osq)
            P.rsq(orr, oss, 1.0 / 128)
            yield
            for h in range(4):
                P.stt("dve", ycat[p][:, 512 + h * 128:512 + (h + 1) * 128], pb[3][:, h * 128:(h + 1) * 128],
                      orr[:, h:h + 1], gz[p][:, h, :], ALU.mult, ALU.mult)
            yield
            for k in range(8):
                P.tr(ptp[:, k * 128:(k + 1) * 128], ycat[p][:, k * 128:(k + 1) * 128], identB)
            yield
            P.cp("act", yT, ptp)
            yield
            xo = x1t
            for half in range(2):
                pbo = pb[2 + half]
                for k in range(8):
                    P.mm(pbo, yT[:, k * 128:(k + 1) * 128], w_outB[:, k, half * 512:(half + 1) * 512],
                         start=(k == 0), stop=(k == 7))
            yield
            for half in range(2):
                P.tt("dve", xo[:, half * 512:(half + 1) * 512], pb[2 + half], g1R[:, half * 512:(half + 1) * 512], ALU.mult)
            yield
            P.tt("dve", xo, xo, xs, ALU.add)
            m = t - NPRE
            P.dma("sp", x1_d[m * 128:(m + 1) * 128, :], xo, "x1st")
            if "d_x1" in dbg:
                P.dma("sp", dbg["d_x1"][m * 128:(m + 1) * 128, :], xo, "dbg")
            if "d_ycat" in dbg:
                P.cp("dve", hf, ycat[p])
                P.dma("sp", dbg["d_ycat"][m * 128:(m + 1) * 128, :], hf, "dbg")
            yield

    def interleave(*gs):
        gens = [g for g in gs if g is not None]
        while gens:
            for g in list(gens):
                try:
                    next(g)
                except StopIteration:
                    gens.remove(g)

    def interleave_bal(facts):
        facts = [f for f in facts if f is not None]
        P.dry = True
        totals = [sum(1 for _ in f()) + 1 for f in facts]
        P.dry = False
        gens = [f() for f in facts]
        done = [0] * len(gens)
        live = list(range(len(gens)))
        while live:
            i = min(live, key=lambda i_: (done[i_] + 0.5) / totals[i_])
            try:
                next(gens[i])
            except StopIteration:
                live.remove(i)
            done[i] += 1

    if PIPE:
        for t in range(NT + 1):
            interleave_bal([(lambda t=t: stage1(t)) if t < NT else None,
                            (lambda t=t: gmlp(t)) if (t < NT and t >= NPRE) else None,
                            (lambda t=t: stage2(t - 1)) if t >= 1 else None])
    else:
        for t in range(NT):
            for _ in stage1(t):
                pass
            if t >= NPRE:
                for _ in gmlp(t):
                    pass
            for _ in stage2(t):
                pass

    P.emit()
    s1.close()
    if KSTOP <= 2:
        P.final_wait()
        return nc

    C = cap
    NSLOT = NE * C
    BIG = 1.0e6
    xg_t = nc.dram_tensor("xg", [NSLOT, D], BF16, kind="Internal")
    xg_d = V(xg_t.ap(), [Buf("xg")])
    yy_t = nc.dram_tensor("yy", [NSLOT, D], BF16, kind="Internal")
    yy_d = V(yy_t.ap(), [Buf("yy")])
    I32 = mybir.dt.int32
    U32 = mybir.dt.uint32

    s2p = ExitStack()
    slotI = sb("slotI", [128, NMAIN, 4], I32, st=s2p)
    wk = sb("wk", [128, NMAIN, 4], F32, st=s2p)

    s2 = ExitStack()

    def sb2(name, shape, dt=F32):
        return sb(name, shape, dt, st=s2)

    mod2a = sb2("mod2a", [128, 2 * D])
    P.dma("sp", mod2a, mod2_d[:, 0:2 * D], "mod2a")
    sh2 = mod2a[:, 0:D]
    A2 = mod2a[:, D:2 * D]
    wrF = sb2("wrF", [128, 8, NE])
    P.dma("sp", wrF, w_router.re("(k p) n -> p k n", p=128), "wrF")
    brR = sb2("brR", [128, NE])
    P.dma("sp", brR, bcast_rows(b_router, NE), "brR")
    iotaE = sb2("iotaE", [128, NE])
    P.dma("sp", iotaE, consts2, "iotaE")
    striuB = sb2("striuB", [128, 128], BF16)
    P.tt("dve", striuB, triu, identF, ALU.subtract)
    maskacc = sb2("maskacc", [128, NE], BF16)
    P.memset("dve", maskacc, 0.0)
    def pp(name, shape, dt=F32):
        return [sb2("%s_%d" % (name, i), shape, dt) for i in range(2)]

    slotI_t = [V(slotI.ap[:, s_, :], [Buf("slotI%d" % s_)]) for s_ in range(NMAIN)]
    wk_t = [V(wk.ap[:, s_, :], [Buf("wk%d" % s_)]) for s_ in range(NMAIN)]
    x2 = pp("x2", [128, D])
    h2f_ = pp("h2f", [128, D])
    junk2_ = pp("junk2", [128, D])
    h2b = pp("h2b", [128, D], BF16)
    h2Tf_ = pp("h2Tf", [128, 8, 128])
    ssq2_ = pp("ssq2", [128, 1])
    rstd2_ = pp("rstd2", [128, 1])
    lg_ = pp("lg", [128, NE])
    top8_ = pp("top8", [128, 8])
    idx8_ = pp("idx8", [128, 8], U32)
    ekf_ = pp("ekf", [128, 4])
    msk_ = pp("msk", [128, NE], BF16)
    posS_ = pp("posS", [128, NE])
    tmp32_ = pp("tmp32", [128, NE])
    pk_ = pp("pk", [128, 4])
    val_ = pp("val", [128, 4])
    slotf_ = pp("slotf", [128, 4])
    ex4_ = pp("ex4", [128, 4])
    nmx_ = pp("nmx", [128, 1])
    den_ = pp("den", [128, 1])

    def tile2a(s):
        q = s % 2
        xs, hb2, h2f, junk2, h2Tf = x2[q], h2b[q], h2f_[q], junk2_[q], h2Tf_[q]
        ssq2, rstd2, lg, top8, idx8, ekf, msk = ssq2_[q], rstd2_[q], lg_[q], top8_[q], idx8_[q], ekf_[q], msk_[q]
        posS, tmp32, pk, val, slotf, ex4, nmx, den = posS_[q], tmp32_[q], pk_[q], val_[q], slotf_[q], ex4_[q], nmx_[q], den_[q]
        pA, pB, pC = (pb[0], pb[1], pb[2]) if q == 0 else (pb[4], pb[5], pb[6])
        sI, wkt = slotI_t[s], wk_t[s]
        P.dma("sp", xs, x1_d[s * 128:(s + 1) * 128, :], "x2_%d" % q)
        P.memset("dve", ssq2, 0.0)
        P.act(junk2, xs, AF.Square, accum=ssq2)
        yield
        P.rsq(rstd2, ssq2, 1.0 / D)
        yield
        P.stt("dve", h2f, xs, rstd2, A2, ALU.mult, ALU.mult)
        yield
        P.tt("dve", h2f, h2f, sh2, ALU.add)
        yield
        P.cp("act", hb2, h2f)
        for half, pbh in enumerate((pA, pB)):
            for k in range(4):
                kk = half * 4 + k
                P.tr(pbh[:, k * 128:(k + 1) * 128], h2f[:, kk * 128:(kk + 1) * 128], identF)
        yield
        P.cp("act", h2Tf[:, 0:4, :], f3(pA))
        P.cp("dve", h2Tf[:, 4:8, :], f3(pB))
        yield
        for k in range(8):
            P.mm(pC[:, 0:NE], h2Tf[:, k, :], wrF[:, k, :], start=(k == 0), stop=(k == 7))
        yield
        P.tt("dve", lg, pC[:, 0:NE], brR, ALU.add)
        yield
        P.add("dve", lambda e: e.max(out=top8.ap, in_=lg.ap), [lg], [top8])
        yield
        P.add("dve", lambda e: e.max_index(out=idx8.ap, in_max=top8.ap, in_values=lg.ap), [lg, top8], [idx8])
        P.ts("dve", msk, lg, top8[:, 3:4], ALU.is_ge)
        P.ts("dve", nmx, top8[:, 0:1], -1.0, ALU.mult)
        yield
        P.mm(pC[:, NE:2 * NE], striuB, msk, start=True, stop=False)
        P.mm(pC[:, NE:2 * NE], onesB, maskacc, start=False, stop=True)
        P.tt("dve", maskacc, maskacc, msk, ALU.add)
        P.cp("dve", ekf, idx8[:, 0:4])
        P.act(ex4, top8[:, 0:4], AF.Exp, bias=nmx[:, 0:1])
        yield
        P.cp("act", posS, pC[:, NE:2 * NE])
        P.rsum("dve", den, ex4)
        yield
        P.add("dve", lambda e: e.reciprocal(den.ap, den.ap), [den], [den])
        for k in range(4):
            P.stt("dve", tmp32, iotaE, ekf[:, k:k + 1], posS, ALU.is_equal, ALU.mult)
            P.rsum("dve", pk[:, k:k + 1], tmp32)
            yield
        P.ts("dve", val, pk, float(C), ALU.is_lt)
        P.stt("dve", slotf, ekf, float(C), pk, ALU.mult, ALU.add)
        yield
        P.ts("dve", slotf, slotf, -BIG, ALU.add)
        P.ts("dve", ex4, ex4, den[:, 0:1], ALU.mult)
        yield
        P.tt("dve", slotf, slotf, val, ALU.mult)
        P.tt("dve", wkt, ex4, val, ALU.mult)
        yield
        P.ts("dve", slotf, slotf, BIG, ALU.add)
        yield
        P.cp("dve", sI, slotf)
        yield
        for k in range(4):
            def sc(e, k=k, s=s, hb2=hb2):
                return e.indirect_dma_start(
                    out=xg_d.ap, out_offset=bass.IndirectOffsetOnAxis(ap=slotI[:, s, k:k + 1].ap, axis=0),
                    in_=hb2.ap, in_offset=None, bounds_check=P.bc_reg(e, NSLOT - 1), oob_is_err=False)
            P.add("pool", sc, [sI, hb2], [xg_d], dkey="xgsc%d" % q)
        yield

    for s in range(0, NMAIN, 2):
        interleave(tile2a(s), tile2a(s + 1) if s + 1 < NMAIN else None)
    NSB = C // 512
    flags_t = nc.dram_tensor("flags", [1, NSB * NE], I32, kind="Internal")
    flags_d = V(flags_t.ap(), [Buf("flags")])
    P.flags_ap = flags_t.ap()
    P.mm(pb[3][:, 0:NE], onesB, maskacc)
    cntS = sb2("cntS", [128, NE])
    P.cp("act", cntS, pb[3][:, 0:NE])
    flagf = sb2("flagf", [128, NSB * NE])
    for sbk in range(NSB):
        P.ts("dve", flagf[:, sbk * NE:(sbk + 1) * NE], cntS, float(sbk * 512), ALU.is_gt)
    flagt = sb2("flagt", [128, NE])
    P.cp("dve", flagt, flagf[:, 0:NE])
    P.tt("dve", flagf[:, 0:NE - 1], flagt[:, 0:NE - 1], flagt[:, 1:NE], ALU.max)
    flagI = sb2("flagI", [128, NSB * NE], I32)
    P.cp("dve", flagI, flagf)
    P.dma("sp", flags_d, flagI[0:1, :], "flags")
    P.emit()
    s2.close()

    s2 = ExitStack()
    bguT = sb2("bguT", [128, 16, NE])
    s2t = ExitStack()
    bguRows = sb("bguRows", [NE, 2 * D], F32, st=s2t)
    P.dma("sp", bguRows, b_gu, "bguRows")
    for c in range(16):
        P.tr(pb[c % 2][:, 0:NE], bguRows[:, c * 128:(c + 1) * 128], identF[0:NE, 0:NE])
        P.cp("dve", bguT[:, c, :], pb[c % 2][:, 0:NE])
    P.emit()
    s2t.close()
    ones1 = sb2("ones1", [1, 128], BF16)
    P.memset("dve", ones1, 1.0)
    wguB = [sb2("wguB%d" % i, [128, 8, 2 * D], BF16) for i in range(2)]
    wdB = [sb2("wdB%d" % i, [128, 8, D], BF16) for i in range(2)]
    bdr = [sb2("bdr%d" % i, [1, D], BF16) for i in range(2)]
    xr = [sb2("xr%d" % i, [128, D], BF16) for i in range(4)]
    xgT = [sb2("xgT%d" % i, [128, 8, 512], BF16) for i in range(2)]
    actT = [sb2("actT%d" % i, [128, 8, 512], BF16) for i in range(2)]
    actK = [[V(a_.ap[:, k_, :], [Buf("actT%d_%d" % (i_, k_))]) for k_ in range(8)] for i_, a_ in enumerate(actT)]
    gq = [sb2("gq%d" % i, [128, 512]) for i in range(2)]
    uq = [sb2("uq%d" % i, [128, 512]) for i in range(2)]
    sg = sb2("sg0", [128, 512])
    tq = sb2("tq0", [128, 512])
    ysb = [sb2("ysb%d" % i, [128, D], BF16) for i in range(2)]

    w_gu_v = w_gu.re("e (k p) n -> e p k n", p=128)
    w_down_v = w_down.re("e (k p) n -> e p k n", p=128)

    stg = [sb2("stg%d" % i, [128, 2048]) for i in range(2)]

    def load_expert(e, slot):
        for k in range(0, 4, 2):
            P.dma("pool", wguB[slot][:, k:k + 2, :], w_gu_v[e, :, k:k + 2, :], "wgu%d" % slot)
        P.dma("pool", bdr[slot], b_down[e:e + 1, :], "bdr%d" % slot)

    def staged_chunks(e, slot):
        ch = []
        for k in range(4, 8):
            ch.append((w_gu_v[e, :, k, :], wguB[slot][:, k, :], False))
        for k in range(0, 8, 2):
            ch.append((w_down_v[e, :, k:k + 2, :], wdB[slot][:, k:k + 2, :], True))
        return ch

    def stream_steps(e, slot):
        ch = staged_chunks(e, slot)
        n = len(ch)
        acts = []
        for c in range(n + 2):
            def act_(c=c):
                if 0 <= c - 2 < n:
                    src, dst, two = ch[c - 2]
                    i = (c - 2) % 2
                    sv = stg[i].re("p (a n) -> p a n", a=2) if two else stg[i]
                    P.cp("dve", dst, sv)
                if c < n:
                    src, dst, two = ch[c]
                    i = c % 2
                    sv = stg[i].re("p (a n) -> p a n", a=2) if two else stg[i]
                    P.dma("act", sv, src, "stg%d" % i)
            acts.append(act_)
        return acts

    load_expert(0, 0)
    for a_ in stream_steps(0, 0):
        a_()
    nblk = [0]
    nsb = 0

    def row_loads(e, sbk):
        r0_ = e * C + sbk * 512
        rows = []
        for j in range(4):
            xrow = xr[nblk[0] % 4]
            P.dma("sp", xrow, xg_d[r0_ + j * 128:r0_ + (j + 1) * 128, :], "xr%d" % (nblk[0] % 4))
            nblk[0] += 1
            rows.append(xrow)
        return rows

    def row_T(rows, j, xT_):
        for k in range(8):
            P.tr(ptp[:, k * 128:(k + 1) * 128], rows[j][:, k * 128:(k + 1) * 128], identB)
        P.cp("act" if j % 2 == 0 else "dve", xT_[:, :, j * 128:(j + 1) * 128], ptp.re("p (k t) -> p k t", k=8))

    P.cond = (0,) if use_skip else None
    rows0 = row_loads(0, 0)
    for j in range(4):
        row_T(rows0, j, xgT[0])
    P.cond = None
    for e in range(n_exp):
        slot = e % 2
        bsteps = []
        if e + 1 < n_exp:
            load_expert(e + 1, 1 - slot)
            bsteps = stream_steps(e + 1, 1 - slot)
        wg = wguB[slot]
        wd = wdB[slot]
        xT = xgT[e % 2]
        for sbk in range(C // 512):
            if use_skip:
                P.cond = (e,) if sbk == 0 else tuple(k_ * NE + e for k_ in range(1, sbk + 1))
            else:
                P.cond = None
            aT = actT[nsb % 2]
            aK = actK[nsb % 2]
            nsb += 1
            r0 = e * C + sbk * 512
            nxt = None
            if sbk == 0:
                if e + 1 < n_exp:
                    nxt = row_loads(e + 1, 0)
            else:
                rws = row_loads(e, sbk)
                for j in range(4):
                    row_T(rws, j, xT)
            for fc in range(8):
                pg_ = pb[fc % 2]
                pu_ = pb[2 + fc % 2]
                for k in range(8):
                    P.mm(pg_, wg[:, k, fc * 128:(fc + 1) * 128], xT[:, k, :], start=(k == 0), stop=(k == 7))
                for k in range(8):
                    P.mm(pu_, wg[:, k, D + fc * 128:D + (fc + 1) * 128], xT[:, k, :], start=(k == 0), stop=(k == 7))
                i2 = fc % 2
                P.ts("dve", gq[i2], pg_, bguT[:, fc, e:e + 1], ALU.add, 7.0, ALU.min)
                P.act(sg, gq[i2], AF.Sigmoid, scale=1.702)
                P.ts("dve", uq[i2], pu_, bguT[:, 8 + fc, e:e + 1], ALU.add, 7.0, ALU.min)
                P.ts("dve", uq[i2], uq[i2], -7.0, ALU.max, 1.0, ALU.add)
                P.tt("dve" if fc == 7 else "pool", tq, gq[i2], sg, ALU.mult)
                P.tt("dve" if fc == 7 else "pool", aK[fc], tq, uq[i2], ALU.mult)
                if bsteps:
                    P.always = True
                    bsteps.pop(0)()
                    P.always = False
            for j in range(4):
                if nxt is not None:
                    row_T(nxt, j, xgT[(e + 1) % 2])
                yb_ = ysb[j % 2]
                for half in range(2):
                    py = pb[4 + half]
                    for k in range(8):
                        P.mm(py, aK[k][:, j * 128:(j + 1) * 128], wd[:, k, half * 512:(half + 1) * 512],
                             start=(k == 0), stop=False)
                    P.mm(py, ones1, bdr[slot][:, half * 512:(half + 1) * 512], start=False, stop=True)
                    P.cp("act", yb_[:, half * 512:(half + 1) * 512], py)
                P.dma("sp", yy_d[r0 + j * 128:r0 + (j + 1) * 128, :], yb_, "yst%d" % (j % 2))
            P.cond = None
        while bsteps:
            bsteps.pop(0)()
    P.emit()
    s2.close()

    s2 = ExitStack()
    yg = [[sb2("yg%d_%d" % (i, k), [128, D], BF16) for k in range(4)] for i in range(2)]
    for i in range(2):
        for k in range(4):
            P.memset("dve" if k % 2 == 0 else "pool", yg[i][k], 0.0)
    g2R = sb2("g2R", [128, D])
    P.dma("sp", g2R, mod2_d[:, 2 * D:3 * D], "g2R")
    nfR = sb2("nfR", [128, D])
    P.dma("sp", nfR, bcast_rows(norm_f_w, D), "nfR")
    x2 = [sb2("x2c_%d" % i, [128, D]) for i in range(2)]
    acc_ = [sb2("acc%d" % i, [128, D]) for i in range(2)]
    xo2_ = [sb2("xo2_%d" % i, [128, D]) for i in range(2)]
    junk2_ = [sb2("junk2c%d" % i, [128, D]) for i in range(2)]
    ssq2_ = [sb2("ssq2c%d" % i, [128, 1]) for i in range(2)]
    rstd2_ = [sb2("rstd2c%d" % i, [128, 1]) for i in range(2)]

    def tile2c(s):
        q = s % 2
        xs, ygs, acc, xo, junk2, ssq2, rstd2 = x2[q], yg[q], acc_[q], xo2_[q], junk2_[q], ssq2_[q], rstd2_[q]
        sI, wkt = slotI_t[s], wk_t[s]
        P.dma("sp", xs, x1_d[s * 128:(s + 1) * 128, :], "x2c_%d" % q)
        for k in range(4):
            def ga(e, k=k, s=s, ygs=ygs):
                return e.indirect_dma_start(
                    out=ygs[k].ap, out_offset=None, in_=yy_d.ap,
                    in_offset=bass.IndirectOffsetOnAxis(ap=slotI[:, s, k:k + 1].ap, axis=0),
                    bounds_check=P.bc_reg(e, NSLOT - 1), oob_is_err=False)
            P.add("pool", ga, [sI, yy_d], [ygs[k]], dkey="yg%d_%d" % (q, k))
        yield
        P.ts("dve", acc, ygs[0], wkt[:, 0:1], ALU.mult)
        yield
        for k in range(1, 4):
            P.stt("dve", acc, ygs[k], wkt[:, k:k + 1], acc, ALU.mult, ALU.add)
            yield
        P.tt("dve", xo, acc, g2R, ALU.mult)
        yield
        P.tt("dve", xo, xo, xs, ALU.add)
        P.memset("dve", ssq2, 0.0)
        yield
        P.act(junk2, xo, AF.Square, accum=ssq2)
        yield
        P.rsq(rstd2, ssq2, 1.0 / D)
        yield
        P.stt("dve", xo, xo, rstd2, nfR, ALU.mult, ALU.mult)
        yield
        P.dma("sp", out_d[s * 128:(s + 1) * 128, :], xo, "out%d" % q)
        yield

    for s in range(0, NMAIN, 2):
        interleave(tile2c(s), tile2c(s + 1) if s + 1 < NMAIN else None)
    P.emit()
    P.final_wait()
    s2.close()
    s2p.close()
    es.close()
    return nc


def make_consts():
    c = np.zeros((128, 512 + NE), np.float32)
    c[:, 512:] = np.arange(NE, dtype=np.float32)[None, :]
    i = np.arange(128)
    c[:, 0:128] = np.eye(128, dtype=np.float32)
    c[:, 128:256] = (i[None, :] >= i[:, None])
    c[:, 256:384] = (i[None, :] <= i[:, None])
    c[:, 384:512] = (i[None, :] < i[:, None])
    return c


def core_inputs(inp, b, half, npre, nmain):
    x = inp["x"]
    L = 0
    f = np.ascontiguousarray
    if half == 0:
        xs = np.concatenate([x[b, :npre * 128], x[b, :nmain * 128]], axis=0)
        flag = 0.0
    else:
        xs = x[b, :(npre + nmain) * 128]
        flag = 1.0
    m = {
        "x_seq": f(xs),
        "flag": np.full((128, 1), flag, np.float32),
        "c_row": f(inp["c"][b].reshape(8, 128)),
        "consts": make_consts(),
        "w_ada": f(inp["w_ada"][L]),
        "b_ada": f(inp["b_ada"][L].reshape(1, -1)),
        "norm1_w": f(inp["norm1_w"][L].reshape(1, -1)),
        "w_in": f(inp["w_in"][L]),
        "gm_vnorm_w": f(inp["gm_vnorm_w"][L].reshape(1, -1)),
        "gm_w_spatial": f(inp["gm_w_spatial"][L]),
        "gm_b_spatial": f(inp["gm_b_spatial"][L]),
        "gdn_conv_w": f(inp["gdn_conv_w"][L]),
        "gdn_a_log": f(inp["gdn_a_log"][L].reshape(1, -1)),
        "gdn_dt_bias": f(inp["gdn_dt_bias"][L].reshape(1, -1)),
        "gdn_onorm_w": f(inp["gdn_onorm_w"][L].reshape(1, -1)),
        "w_out": f(inp["w_out"][L]),
        "norm2_w": f(inp["norm2_w"][L].reshape(1, -1)),
        "w_router": f(inp["w_router"][L]),
        "b_router": f(inp["b_router"][L].reshape(1, -1)),
        "w_gu": f(inp["w_gu"][L]),
        "b_gu": f(inp["b_gu"][L]),
        "w_down": f(inp["w_down"][L]),
        "b_down": f(inp["b_down"][L]),
        "norm_f_w": f(inp["norm_f_w"].reshape(1, -1)),
    }
    return m


def kernel(**inputs):
    inp = {k: np.asarray(v, dtype=np.float32) for k, v in inputs.items()}
    B, S, _ = inp["x"].shape
    nh = S // 2 // 128
    nc = build(nh, nh)
    in_maps = []
    for c in range(8):
        in_maps.append(core_inputs(inp, c // 2, c % 2, nh, nh))
    res = run_bass_kernel_spmd(nc, in_maps, core_ids=list(range(8)))
    out = np.empty((B, S, D), np.float32)
    for c in range(8):
        b, half = c // 2, c % 2
        out[b, half * (S // 2):(half + 1) * (S // 2)] = res.results[c]["out"]
    return out
```

```python
from contextlib import ExitStack
import numpy as np
import concourse.bass as bass
import concourse.mybir as mybir
from concourse.bass_utils import run_bass_kernel_spmd

F32 = mybir.dt.float32
BF16 = mybir.dt.bfloat16
AF = mybir.ActivationFunctionType
ALU = mybir.AluOpType
AX = mybir.AxisListType

D = 1024
NCOLS = 3080
NE = 32
EPS = 1e-6


class Buf:
    __slots__ = ("name", "w", "r", "excl")

    def __init__(self, name, excl=False):
        self.name = name
        self.w = {}
        self.r = {}
        self.excl = excl


class V:
    __slots__ = ("ap", "bufs")

    def __init__(self, ap, bufs):
        self.ap = ap
        self.bufs = bufs

    def __getitem__(self, key):
        return V(self.ap[key], self.bufs)

    def re(self, s, **kw):
        return V(self.ap.rearrange(s, **kw), self.bufs)

    def bc(self, shape):
        return V(self.ap.to_broadcast(shape), self.bufs)


class Op:
    __slots__ = ("eng", "fn", "deps", "idx", "inc", "val", "dkey", "cond", "dn", "always")


COMPUTE = ("pe", "act", "dve", "pool")


class Prog:
    def __init__(self, nc):
        self.nc = nc
        self.ops = {e: [] for e in COMPUTE + ("sp",)}
        self.emitted = {e: 0 for e in COMPUTE + ("sp",)}
        self.dcount = {}
        self.sems = {}
        self.es = ExitStack()
        self.seen = {e: {} for e in COMPUTE + ("sp",)}
        self.ninc = {e: 0 for e in COMPUTE}
        self.barrier = None
        self.regs = {}
        self.cond = None
        self.always = False
        self.dry = False
        self.flags_ap = None

    def bc_reg(self, e, val):
        if val not in self.regs:
            reg = e.alloc_register("bcr%d" % val)
            e.reg_mov(reg, val)
            self.regs[val] = reg
        return self.regs[val]

    def sem(self, name):
        if name not in self.sems:
            self.sems[name] = self.es.enter_context(self.nc.semaphore("s_" + name.replace(":", "_")))
        return self.sems[name]

    def add(self, eng, fn, reads, writes, dkey=None):
        if self.dry:
            return None
        op = Op()
        op.eng = eng
        op.fn = fn
        op.inc = False
        op.val = None
        op.dkey = dkey
        op.cond = self.cond
        op.always = self.always
        op.dn = None
        op.idx = len(self.ops[eng])
        deps = {}

        def need(src, idx):
            if src == "pe" and eng == "pe" and dkey is None:
                return
            if deps.get(src, -1) < idx:
                deps[src] = idx

        rb = [b for v in reads if isinstance(v, V) for b in v.bufs]
        wb = [b for v in writes if isinstance(v, V) for b in v.bufs]
        for b in rb:
            for s, i in b.w.items():
                need(s, i)
            if b.excl:
                for s, i in b.r.items():
                    if s != eng:
                        need(s, i)
        for b in wb:
            for s, i in b.w.items():
                need(s, i)
            for s, i in b.r.items():
                need(s, i)
        op.deps = deps
        if dkey is not None:
            self.dcount[dkey] = self.dcount.get(dkey, 0) + 1
            op.dn = self.dcount[dkey]
            me = ("D:" + dkey, self.dcount[dkey])
        else:
            me = (eng, op.idx)
        for b in rb:
            b.r[me[0]] = me[1]
        for b in wb:
            b.w[me[0]] = me[1]
        self.ops[eng].append(op)
        return op

    def emit(self):
        nc = self.nc
        for e in self.ops:
            for op in self.ops[e][self.emitted[e]:]:
                for s, i in op.deps.items():
                    if not s.startswith("D:") and i >= self.emitted[s]:
                        self.ops[s][i].inc = True
        for e in COMPUTE:
            if len(self.ops[e]) > self.emitted[e]:
                self.ops[e][-1].inc = True
        for e in COMPUTE:
            for op in self.ops[e][self.emitted[e]:]:
                if op.inc and op.dkey is None:
                    self.ninc[e] += 1
                    op.val = self.ninc[e]
        engs = {"pe": nc.tensor, "act": nc.scalar, "dve": nc.vector, "pool": nc.gpsimd, "sp": nc.sync}
        for k in self.dcount:
            self.sem("D:" + k)
        for e in COMPUTE:
            self.sem(e)
        barrier = self.barrier
        start = dict(self.emitted)

        def run(ename, engine):
            seen = self.seen[ename]
            ops = self.ops[ename][self.emitted[ename]:]
            plan = []
            first = True
            for op in ops:
                waits = {}
                if first and barrier is not None:
                    for s, v in barrier.items():
                        waits[s] = v
                first = False
                for s, i in op.deps.items():
                    if s.startswith("D:"):
                        v = 16 * i
                    else:
                        if i < start[s]:
                            continue
                        v = self.ops[s][i].val
                        assert v is not None, (s, i)
                    if waits.get(s, 0) < v:
                        waits[s] = v
                wl = []
                for s, v in waits.items():
                    if seen.get(s, 0) >= v:
                        continue
                    seen[s] = v
                    wl.append((s, v))
                plan.append((op, wl))

            def emit_real(group):
                for op, wl in group:
                    for s, v in wl:
                        engine.wait_ge(self.sem(s), v)
                    ins = op.fn(engine)
                    if op.dkey is not None:
                        ins.then_inc(self.sem("D:" + op.dkey), 16)
                    elif op.inc:
                        ins.then_inc(self.sem(ename), 1)

            def emit_skip(group):
                pend_inc = {}
                pend_wait = {}

                def flush():
                    for k, (before, tot) in pend_inc.items():
                        engine.wait_ge(self.sem(k), before)
                        engine.sem_inc(self.sem(k), tot)
                    for s_, v in pend_wait.items():
                        engine.wait_ge(self.sem(s_), v)
                    pend_inc.clear()
                    pend_wait.clear()

                for op, wl in group:
                    if op.always:
                        flush()
                        emit_real([(op, wl)])
                        continue
                    if op.dkey is not None:
                        k = "D:" + op.dkey
                        if k not in pend_inc:
                            pend_inc[k] = [16 * (op.dn - 1), 0]
                        pend_inc[k][1] += 16
                    elif op.inc:
                        if ename not in pend_inc:
                            pend_inc[ename] = [op.val - 1, 0]
                        pend_inc[ename][1] += 1
                    for s_, v in wl:
                        if pend_wait.get(s_, 0) < v:
                            pend_wait[s_] = v
                flush()

            def tag_at(op, depth):
                c = op.cond
                if c is None or len(c) <= depth:
                    return None
                return c[depth]

            def emit_level(items, depth):
                i = 0
                while i < len(items):
                    tag = tag_at(items[i][0], depth)
                    j = i
                    while j < len(items) and tag_at(items[j][0], depth) == tag:
                        j += 1
                    group = items[i:j]
                    if tag is None:
                        emit_real(group)
                    else:
                        key = "cr_" + ename
                        if key not in self.regs:
                            self.regs[key] = engine.alloc_register(key)
                        reg = self.regs[key]
                        engine.reg_load(reg, self.flags_ap[0:1, tag:tag + 1])
                        with engine.If_eq(reg, 0):
                            emit_skip(group)
                        with engine.Else():
                            emit_level(group, depth + 1)
                    i = j

            emit_level(plan, 0)

        with nc.Block() as block:
            @block.tensor
            def _(eng):
                run("pe", eng)

            @block.scalar
            def _(eng):
                run("act", eng)

            @block.vector
            def _(eng):
                run("dve", eng)

            @block.gpsimd
            def _(eng):
                run("pool", eng)

            @block.sync
            def _(eng):
                run("sp", eng)
        for e in self.ops:
            self.emitted[e] = len(self.ops[e])
        b = {}
        for e in COMPUTE:
            if self.ninc[e]:
                b[e] = self.ninc[e]
        for k, c in self.dcount.items():
            b["D:" + k] = 16 * c
        self.barrier = b

    def final_wait(self):
        nc = self.nc
        b = self.barrier
        with nc.Block() as block:
            @block.sync
            def _(eng):
                for s, v in b.items():
                    eng.wait_ge(self.sem(s), v)
        self.es.close()

    def mm(self, out, lhsT, rhs, start=True, stop=True):
        rd = [lhsT, rhs] + ([] if start else [out])
        self.add("pe", lambda e: e.matmul(out.ap, lhsT.ap, rhs.ap, start=start, stop=stop), rd, [out])

    def tr(self, out, in_, ident):
        self.add("pe", lambda e: e.transpose(out.ap, in_.ap, ident.ap), [in_, ident], [out])

    def act(self, out, in_, func, bias=None, scale=1.0, accum=None, eng="act"):
        rd = [in_]
        kw = {}
        if bias is not None:
            kw["bias"] = bias.ap if isinstance(bias, V) else bias
            rd.append(bias)
        if isinstance(scale, V):
            kw["scale"] = scale.ap
            rd.append(scale)
        else:
            kw["scale"] = scale
        wr = [out]
        if accum is not None:
            kw["accum_out"] = accum.ap
            wr.append(accum)
        self.add(eng, lambda e: e.activation(out.ap, in_.ap, func, **kw), rd, wr)

    def tt(self, eng, out, a, b, op):
        self.add(eng, lambda e: e.tensor_tensor(out.ap, a.ap, b.ap, op), [a, b], [out])

    def ts(self, eng, out, a, s1, op0, s2=None, op1=None):
        rd = [a]
        x1 = s1.ap if isinstance(s1, V) else s1
        x2 = s2.ap if isinstance(s2, V) else s2
        if isinstance(s1, V):
            rd.append(s1)
        if isinstance(s2, V):
            rd.append(s2)
        if op1 is None:
            self.add(eng, lambda e: e.tensor_scalar(out.ap, a.ap, x1, None, op0), rd, [out])
        else:
            self.add(eng, lambda e: e.tensor_scalar(out.ap, a.ap, x1, x2, op0, op1), rd, [out])

    def stt(self, eng, out, in0, scalar, in1, op0, op1):
        rd = [in0, in1]
        sc = scalar.ap if isinstance(scalar, V) else scalar
        if isinstance(scalar, V):
            rd.append(scalar)
        self.add(eng, lambda e: e.scalar_tensor_tensor(out.ap, in0.ap, sc, in1.ap, op0, op1), rd, [out])

    def rsq(self, out, in_, scale=1.0):
        self.act(out, in_, AF.Ln, bias=EPS, scale=scale)
        self.act(out, out, AF.Exp, scale=-0.5)

    def cp(self, eng, out, in_):
        if eng == "act":
            self.add(eng, lambda e: e.copy(out.ap, in_.ap), [in_], [out])
        else:
            self.add(eng, lambda e: e.tensor_copy(out.ap, in_.ap), [in_], [out])

    def memset(self, eng, out, val):
        self.add(eng, lambda e: e.memset(out.ap, val), [], [out])

    def rsum(self, eng, out, in_):
        self.add(eng, lambda e: e.reduce_sum(out.ap, in_.ap, AX.X), [in_], [out])

    def dma(self, eng, out, in_, key, slow=False):
        if slow:
            self.add(eng, lambda e: e.dma_start(out.ap, in_.ap, allow_slow_non_contiguous=True), [in_], [out], dkey=key)
        else:
            self.add(eng, lambda e: e.dma_start(out.ap, in_.ap), [in_], [out], dkey=key)


def build(NPRE, NMAIN, debug=False, n_exp=NE, cap=2048, use_skip=True, PIPE=True):
    nc = bass.Bass("TRN2", target_bir_lowering=False)
    P = Prog(nc)
    NT = NPRE + NMAIN
    NTOK = NMAIN * 128

    def din(name, shape):
        t = nc.dram_tensor(name, list(shape), F32, kind="ExternalInput")
        return V(t.ap(), [Buf(name)])

    x_seq = din("x_seq", [NT * 128, D])
    flag_d = din("flag", [128, 1])
    c_row = din("c_row", [8, 128])
    consts_all = din("consts", [128, 512 + NE])
    consts = consts_all[:, 0:512]
    consts2 = consts_all[:, 512:512 + NE]
    w_ada = din("w_ada", [D, 6 * D])
    b_ada = din("b_ada", [1, 6 * D])
    norm1_w = din("norm1_w", [1, D])
    w_in = din("w_in", [D, NCOLS])
    gm_vnorm_w = din("gm_vnorm_w", [1, 512])
    gm_w_spatial = din("gm_w_spatial", [4, 128, 128])
    gm_b_spatial = din("gm_b_spatial", [4, 128])
    gdn_conv_w = din("gdn_conv_w", [4, 1536])
    gdn_a_log = din("gdn_a_log", [1, 4])
    gdn_dt_bias = din("gdn_dt_bias", [1, 4])
    gdn_onorm_w = din("gdn_onorm_w", [1, 128])
    w_out = din("w_out", [D, D])
    norm2_w = din("norm2_w", [1, D])
    w_router = din("w_router", [D, NE])
    b_router = din("b_router", [1, NE])
    w_gu = din("w_gu", [NE, D, 2 * D])
    b_gu = din("b_gu", [NE, 2 * D])
    w_down = din("w_down", [NE, D, D])
    b_down = din("b_down", [NE, D])
    norm_f_w = din("norm_f_w", [1, D])
    out_t = nc.dram_tensor("out", [NTOK, D], F32, kind="ExternalOutput")
    out_d = V(out_t.ap(), [Buf("out")])
    x1_t = nc.dram_tensor("x1s", [NTOK, D], F32, kind="Internal")
    x1_d = V(x1_t.ap(), [Buf("x1s")])
    dbg = {}
    if debug:
        for nm, shp in debug.items():
            t = nc.dram_tensor(nm, list(shp), F32, kind="ExternalOutput")
            dbg[nm] = V(t.ap(), [Buf(nm)])

    es = ExitStack()

    def sb(name, shape, dt=F32, st=None):
        t = (st or es).enter_context(nc.sbuf_tensor(name, list(shape), dt))
        return V(t[:], [Buf(name)])

    def ps(name, shape, dt=F32, st=None):
        t = (st or es).enter_context(nc.psum_tensor(name, list(shape), dt))
        return V(t[:], [Buf(name, excl=True)])

    def bcast_rows(src, n):
        return V(src.ap[0:1, :].to_broadcast([128, n]), src.bufs)

    cst = sb("cst", [128, 512])
    mod1 = sb("mod1", [128, 3 * D])
    identB = sb("identB", [128, 128], BF16)
    onesB = sb("onesB", [128, 128], BF16)
    flagT = sb("flagT", [128, 1])
    identF = cst[:, 0:128]
    triu = cst[:, 128:256]
    tril = cst[:, 256:384]
    stril = cst[:, 384:512]
    sh1 = mod1[:, 0:D]
    A1 = mod1[:, D:2 * D]
    g1R = mod1[:, 2 * D:3 * D]
    mod2_t = nc.dram_tensor("mod2s", [128, 3 * D], F32, kind="Internal")
    mod2_d = V(mod2_t.ap(), [Buf("mod2s")])

    P.dma("sp", cst, consts, "cst")
    P.dma("sp", flagT, flag_d, "flag")
    P.cp("dve", identB, identF)
    P.memset("dve", onesB, 1.0)

    pb = [ps("pb%d" % i, [128, 512]) for i in range(7)]
    ptp = ps("ptp", [128, 1024], BF16)

    s1 = ExitStack()

    def sb1(name, shape, dt=F32):
        return sb(name, shape, dt, st=s1)

    w_inB = sb1("w_inB", [128, 8, NCOLS], BF16)
    w_in_v = w_in.re("(k p) n -> p k n", p=128)
    for k in range(8):
        P.dma("pool", w_inB[:, k, :], w_in_v[:, k, :], "w_inB")
    w_outB = sb1("w_outB", [128, 8, D], BF16)
    w_out_v = w_out.re("(k p) n -> p k n", p=128)
    for k in range(0, 8, 4):
        P.dma("pool", w_outB[:, k:k + 4, :], w_out_v[:, k:k + 4, :], "w_outB")
    s0 = ExitStack()

    def sb0(name, shape, dt=F32):
        return sb(name, shape, dt, st=s0)

    modR = sb0("modR", [128, 6 * D])
    A1t = modR[:, D:2 * D]
    A2t = modR[:, 4 * D:5 * D]
    cT = sb0("cT", [128, 8])
    cact = sb0("cact", [128, 8])
    cactB = sb0("cactB", [128, 8, 128])
    P.dma("sp", cT, c_row.re("k p -> p k"), "cT", slow=True)
    P.act(cact, cT, AF.Silu)
    for k in range(8):
        P.cp("dve", cactB[:, k, :], cact[:, k:k + 1].bc([128, 128]))
    P.dma("sp", modR, bcast_rows(b_ada, 6 * D), "modR")
    wst = [sb0("wst%d" % i, [128, 8, 512]) for i in range(2)]
    w_ada_v = w_ada.re("(k p) n -> p k n", p=128)
    for cb in range(12):
        st = wst[cb % 2]
        P.dma("sp", st, w_ada_v[:, :, cb * 512:(cb + 1) * 512], "wst%d" % (cb % 2))
        pm = pb[cb % 2]
        for k in range(8):
            P.mm(pm, cactB[:, k, :], st[:, k, :], start=(k == 0), stop=(k == 7))
        P.tt("dve", modR[:, cb * 512:(cb + 1) * 512], modR[:, cb * 512:(cb + 1) * 512], pm, ALU.add)
    nwR = sb0("nwR", [128, D])
    P.dma("sp", nwR, bcast_rows(norm1_w, D), "nwR")
    P.stt("dve", A1t, A1t, 1.0, nwR, ALU.add, ALU.mult)
    nwR2 = sb0("nwR2", [128, D])
    P.dma("sp", nwR2, bcast_rows(norm2_w, D), "nwR2")
    P.stt("dve", A2t, A2t, 1.0, nwR2, ALU.add, ALU.mult)
    P.cp("act", mod1, modR[:, 0:3 * D])
    P.dma("sp", mod2_d, modR[:, 3 * D:6 * D], "mod2s")
    P.emit()
    s0.close()
    import os
    KSTOP = int(os.environ.get("KSTOP", "9"))
    KCUT = int(os.environ.get("KCUT", "99"))
    KSUB = int(os.environ.get("KSUB", "99"))
    if KSTOP == 0:
        P.final_wait()
        return nc
    if KSTOP == 1:
        NT = 0

    wsF = sb1("wsF", [128, 4, 128])
    P.dma("sp", wsF, gm_w_spatial.re("g t s -> t g s"), "wsF")
    for g in range(4):
        P.tt("dve", wsF[:, g, :], wsF[:, g, :], tril, ALU.mult)
    wsT = sb1("wsT", [128, 4, 128], BF16)
    for g in range(4):
        P.tr(pb[2][:, g * 128:(g + 1) * 128], wsF[:, g, :], identF)
    P.cp("dve", wsT.re("p g t -> p (g t)"), pb[2])
    bsp = sb1("bsp", [128, 4])
    P.dma("sp", bsp, gm_b_spatial.re("g t -> t g"), "bsp", slow=True)
    vnwR = sb1("vnwR", [128, 512])
    P.dma("sp", vnwR, bcast_rows(gm_vnorm_w, 512), "vnwR")
    convw = sb1("convw", [128, 4, 12])
    for j in range(4):
        P.dma("sp", convw[:, j, :], gdn_conv_w[j, :].re("(c p) -> p c", p=128), "convw", slow=True)
    alog = sb1("alog", [128, 4])
    dtb = sb1("dtb", [128, 4])
    nexpA = sb1("nexpA", [128, 4])
    P.dma("sp", alog, bcast_rows(gdn_a_log, 4), "alog")
    P.dma("sp", dtb, bcast_rows(gdn_dt_bias, 4), "dtb")
    P.act(nexpA, alog, AF.Exp)
    P.ts("dve", nexpA, nexpA, -1.0, ALU.mult)
    onwR = sb1("onwR", [128, 128])
    P.dma("sp", onwR, bcast_rows(gdn_onorm_w, 128), "onwR")

    xt = [sb1("xt%d" % i, [128, D]) for i in range(2)]
    ssq = sb1("ssq", [128, 1])
    rstd = sb1("rstd", [128, 1])
    hf = sb1("hf", [128, D])
    junk = hf
    hb = sb1("hb", [128, D], BF16)
    hT = sb1("hT", [128, D], BF16)
    uS = sb1("uS", [128, 512])
    gvS = sb1("gvS", [128, 512])
    vss = sb1("vss", [128, 4])
    vr = sb1("vr", [128, 4])
    vn = sb1("vn", [128, 512], BF16)
    ycat = [sb1("ycat%d" % i, [128, D], BF16) for i in range(2)]
    yT = sb1("yT", [128, D], BF16)
    cbuf = sb1("cbuf", [128, 12, 131])
    cvaA = sb1("cvaA", [128, 8, 128])
    cvaB = sb1("cvaB", [128, 4, 128])
    cvbA = sb1("cvbA", [128, 8, 128])
    cvbB = sb1("cvbB", [128, 4, 128])
    qkvS = sb1("qkvS", [128, 12, 128])
    sqn = sb1("sqn", [128, 8, 128], BF16)
    sq5 = cvaA[:, 0:4, :].re("p h e -> p (h e)")
    rn = cvbA
    t1 = cvbB
    Din = cvaA[:, 4:8, :]
    DTin = cvaA[:, 4:8, :]
    Dm = cvaB
    qT = sb1("qT", [128, 4, 128], BF16)
    kT = sb1("kT", [128, 4, 128], BF16)
    vTb = sb1("vTb", [128, 4, 128], BF16)
    abx = sb1("abx", [128, 4])
    aex = sb1("aex", [128, 4])
    asp = sb1("asp", [128, 4])
    gS = sb1("gS", [128, 4])
    beta = sb1("beta", [128, 4])
    nbeta = sb1("nbeta", [128, 4])
    gcS = sb1("gcS", [128, 4])
    egct = sb1("egct", [128, 4])
    bg = sb1("bg", [128, 4])
    dlin = sb1("dlin", [128, 4])
    dl = sb1("dl", [128, 4])
    gB = sb1("gB", [128, 4, 128])
    egcB = sb1("egcB", [128, 4, 128])
    DTm = sb1("DTm", [128, 4, 128])
    N0 = [sb1("N0_%d" % i, [128, 4, 128]) for i in range(2)]
    NT0 = [sb1("NT0_%d" % i, [128, 4, 128]) for i in range(2)]
    R0 = [sb1("R0_%d" % i, [128, 4, 128]) for i in range(2)]
    vb = [sb1("vb%d" % i, [128, 4, 128], BF16) for i in range(2)]
    kbg = [sb1("kbg%d" % i, [128, 4, 128], BF16) for i in range(2)]
    kdec = [sb1("kdec%d" % i, [128, 4, 128], BF16) for i in range(2)]
    qgT = [sb1("qgT%d" % i, [128, 4, 128], BF16) for i in range(2)]
    qkTb = [sb1("qkTb%d" % i, [128, 4, 128], BF16) for i in range(2)]
    egl = [sb1("egl%d" % i, [128, 4]) for i in range(2)]
    gz = [sb1("gz%d" % i, [128, 4, 128]) for i in range(2)]
    Pm = sb1("Pm", [128, 4, 128])
    PTm = sb1("PTm", [128, 4, 128])
    RT = sb1("RT", [128, 4, 128])
    TTb = sb1("TTb", [128, 4, 128], BF16)
    uU = sb1("uU", [128, 4, 128])
    osq = uU
    wTb = sb1("wTb", [128, 4, 128], BF16)
    vnew = sb1("vnew", [128, 4, 128], BF16)
    Sf = sb1("Sf", [128, 4, 128])
    Sb = sb1("Sb", [128, 4, 128], BF16)
    szS = sb1("szS", [128, 512])
    oss = sb1("oss", [128, 4])
    orr = sb1("orr", [128, 4])
    x1t = sb1("x1t0", [128, D])

    P.memset("dve", cbuf, 0.0)
    P.memset("dve", Sf, 0.0)
    P.memset("dve", Sb, 0.0)

    def f3(v):
        return v.re("p (h e) -> p h e", h=4)

    def f2(v):
        return v.re("p h e -> p (h e)")

    def gmlp(t):
        p = t % 2
        sq5g = szS
        yield
        yield
        yield

        def proj1(c0):
            for k in range(8):
                P.mm(pb[1], hT[:, k * 128:(k + 1) * 128], w_inB[:, k, c0:c0 + 512], start=(k == 0), stop=(k == 7))

        proj1(512)
        yield
        P.act(gvS, pb[1], AF.Gelu)
        yield
        proj1(0)
        P.tt("pool", sq5g, gvS, gvS, ALU.mult)
        yield
        P.act(uS, pb[1], AF.Gelu)
        P.rsum("dve", vss, f3(sq5g))
        yield
        P.rsq(vr, vss, 1.0 / 128)
        yield
        for g in range(4):
            P.stt("dve", vn[:, g * 128:(g + 1) * 128], gvS[:, g * 128:(g + 1) * 128], vr[:, g:g + 1],
                  vnwR[:, g * 128:(g + 1) * 128], ALU.mult, ALU.mult)
            if g % 2 == 1:
                yield
        for g in range(4):
            P.mm(pb[1][:, g * 128:(g + 1) * 128], wsT[:, g, :], vn[:, g * 128:(g + 1) * 128])
        yield
        for g in range(4):
            P.stt("dve", ycat[p][:, g * 128:(g + 1) * 128], pb[1][:, g * 128:(g + 1) * 128], bsp[:, g:g + 1],
                  uS[:, g * 128:(g + 1) * 128], ALU.add, ALU.mult)
            if g % 2 == 1:
                yield
        proj1(2560)
        yield
        P.act(szS, pb[1], AF.Silu)
        yield
        P.tt("pool", gz[p], f3(szS), onwR[:, None, :].bc([128, 4, 128]), ALU.mult)
        yield

    def stage1(t):
        main = t >= NPRE
        p = t % 2
        xs = xt[p]
        P.dma("sp", xs, x_seq[t * 128:(t + 1) * 128, :], "xt%d" % p)
        P.memset("dve", ssq, 0.0)
        P.act(junk, xs, AF.Square, accum=ssq)
        P.rsq(rstd, ssq, 1.0 / D)
        yield
        P.stt("dve", hf, xs, rstd, A1, ALU.mult, ALU.mult)
        P.tt("dve", hb, hf, sh1, ALU.add)
        yield
        for k in range(8):
            P.tr(ptp[:, k * 128:(k + 1) * 128], hb[:, k * 128:(k + 1) * 128], identB)
        P.cp("act", hT, ptp)
        yield

        def proj_tok(pso, c0, n):
            for k in range(8):
                P.mm(pso, hT[:, k * 128:(k + 1) * 128], w_inB[:, k, c0:c0 + n], start=(k == 0), stop=(k == 7))

        g0 = 0 if (main or t == NPRE - 1) else 1
        for grp in range(g0, 3):
            pq = (pb[0], pb[4], pb[5])[grp]
            for c in range(4):
                col = 1024 + (grp * 4 + c) * 128
                for k in range(8):
                    P.mm(pq[:, c * 128:(c + 1) * 128], w_inB[:, k, col:col + 128], hT[:, k * 128:(k + 1) * 128],
                         start=(k == 0), stop=(k == 7))
            P.cp("act", cbuf[:, grp * 4:(grp + 1) * 4, 3:131], f3(pq))
            yield
        for k in range(8):
            P.mm(pb[4][:, 0:8], hT[:, k * 128:(k + 1) * 128], w_inB[:, k, 3072:3080], start=(k == 0), stop=(k == 7))
        c0 = 0 if main else 4

        def cwA(j):
            return convw[:, j, c0:8, None].bc([128, 8 - c0, 128])

        def cwB(j):
            return convw[:, j, 8:12, None].bc([128, 4, 128])

        P.tt("dve", cvaA[:, c0:8, :], cbuf[:, c0:8, 0:128], cwA(0), ALU.mult)
        P.tt("pool", cvaB, cbuf[:, 8:12, 0:128], cwB(0), ALU.mult)
        P.tt("dve", cvbA[:, c0:8, :], cbuf[:, c0:8, 1:129], cwA(1), ALU.mult)
        P.tt("pool", cvbB, cbuf[:, 8:12, 1:129], cwB(1), ALU.mult)
        yield
        P.tt("dve", cvaA[:, c0:8, :], cvaA[:, c0:8, :], cvbA[:, c0:8, :], ALU.add)
        P.tt("pool", cvaB, cvaB, cvbB, ALU.add)
        P.tt("dve", cvbA[:, c0:8, :], cbuf[:, c0:8, 2:130], cwA(2), ALU.mult)
        P.tt("pool", cvbB, cbuf[:, 8:12, 2:130], cwB(2), ALU.mult)
        yield
        P.tt("dve", cvaA[:, c0:8, :], cvaA[:, c0:8, :], cvbA[:, c0:8, :], ALU.add)
        P.tt("pool", cvaB, cvaB, cvbB, ALU.add)
        P.tt("dve", cvbA[:, c0:8, :], cbuf[:, c0:8, 3:131], cwA(3), ALU.mult)
        P.tt("pool", cvbB, cbuf[:, 8:12, 3:131], cwB(3), ALU.mult)
        yield
        P.tt("dve", cvaA[:, c0:8, :], cvaA[:, c0:8, :], cvbA[:, c0:8, :], ALU.add)
        P.tt("pool", cvaB, cvaB, cvbB, ALU.add)
        P.cp("act", cbuf[:, :, 0:3], cbuf[:, :, 128:131])
        if t == NPRE - 1:
            P.ts("dve", cbuf[:, :, 0:3], cbuf[:, :, 0:3], flagT[:, 0:1], ALU.mult)
        P.act(qkvS[:, c0:8, :], cvaA[:, c0:8, :], AF.Silu)
        P.act(qkvS[:, 8:12, :], cvaB, AF.Silu)
        yield
        P.act(beta, pb[4][:, 4:8], AF.Sigmoid)
        P.tt("dve", abx, pb[4][:, 0:4], dtb, ALU.add)
        P.act(aex, abx, AF.Exp)
        P.act(asp, aex, AF.Ln, bias=1.0)
        yield
        P.tt("dve", sqn[:, c0:8, :], qkvS[:, c0:8, :], qkvS[:, c0:8, :], ALU.mult)
        P.tt("dve", gS, asp, nexpA, ALU.mult)
        P.ts("dve", nbeta, beta, -1.0, ALU.mult)
        yield
        if main:
            P.mm(pb[0], onesB, f2(sqn[:, 0:4, :]))
        P.mm(pb[5], onesB, f2(sqn[:, 4:8, :]))
        P.mm(pb[4][:, 8:12], triu, gS)
        P.cp("act", gB, gS[:, :, None].bc([128, 4, 128]))
        yield
        if main:
            P.rsq(f2(rn[:, 0:4, :]), pb[0])
        P.rsq(f2(rn[:, 4:8, :]), pb[5])
        P.cp("dve", gcS, pb[4][:, 8:12])
        for h in range(4):
            P.mm(pb[5][:, h * 128:(h + 1) * 128], gB[:, h, :], triu)
        yield
        if main:
            P.stt("dve", qT, qkvS[:, 0:4, :], float(128 ** -0.5), rn[:, 0:4, :], ALU.mult, ALU.mult)
        P.tt("dve", kT, qkvS[:, 4:8, :], rn[:, 4:8, :], ALU.mult)
        P.cp("act", vTb, qkvS[:, 8:12, :])
        P.act(f2(egcB), pb[5], AF.Exp)
        P.act(egct, gcS, AF.Exp)
        yield
        for h in range(4):
            P.tr(ptp[:, h * 128:(h + 1) * 128], kT[:, h, :], identB)
        for h in range(4):
            P.tr(ptp[:, 512 + h * 128:512 + (h + 1) * 128], vTb[:, h, :], identB)
        P.tt("dve", bg, beta, egct, ALU.mult)
        P.tt("dve", dlin, f3(pb[5])[:, :, 127], gcS, ALU.subtract)
        P.act(dl, dlin, AF.Exp)
        yield
        for h in range(4):
            P.mm(pb[4][:, h * 128:(h + 1) * 128], kT[:, h, :], kT[:, h, :])
        for h in range(4):
            P.ts("dve", Din[:, h, :], pb[5][:, h * 128:(h + 1) * 128], gcS[:, h:h + 1], ALU.subtract, 0.0, ALU.max)
        P.act(Dm, Din, AF.Exp, scale=-1.0)
        P.tt("pool", Dm, Dm, stril[:, None, :].bc([128, 4, 128]), ALU.mult)
        yield
        ktok = f3(ptp[:, 0:512])
        vtok = f3(ptp[:, 512:1024])
        P.tt("dve", kbg[p], ktok, bg[:, :, None].bc([128, 4, 128]), ALU.mult)
        P.tt("dve", kdec[p], ktok, dl[:, :, None].bc([128, 4, 128]), ALU.mult)
        P.tt("dve", vb[p], vtok, beta[:, :, None].bc([128, 4, 128]), ALU.mult)
        P.cp("act", egl[p], egcB[:, :, 127])
        yield
        P.tt("dve", t1, f3(pb[4]), Dm, ALU.mult)
        P.tt("dve", N0[p], t1, nbeta[:, :, None].bc([128, 4, 128]), ALU.mult)
        yield
        if main:
            for h in range(4):
                P.ts("dve", DTin[:, h, :], pb[5][:, h * 128:(h + 1) * 128], gcS[:, h:h + 1], ALU.subtract, 0.0, ALU.min)
            P.act(DTm, DTin, AF.Exp)
        yield
        for h in range(4):
            P.tr(pb[0][:, h * 128:(h + 1) * 128], N0[p][:, h, :], identF)
        if main:
            P.tt("pool", DTm, DTm, triu[:, None, :].bc([128, 4, 128]), ALU.mult)
            for h in range(4):
                P.mm(pb[4][:, h * 128:(h + 1) * 128], kT[:, h, :], qT[:, h, :])
        yield
        P.cp("act", f2(NT0[p]), pb[0])
        P.tt("dve", R0[p], f3(pb[0]), identF[:, None, :].bc([128, 4, 128]), ALU.add)
        if main:
            P.tt("pool", qgT[p], qT, egcB, ALU.mult)
            P.tt("dve", qkTb[p], f3(pb[4]), DTm, ALU.mult)
        yield

    def stage2(t):
        main = t >= NPRE
        p = t % 2
        xs = xt[p]
        for lvl in range(1, 7):
            last = lvl == 6
            Pc = N0[p] if lvl == 1 else Pm
            PTc = NT0[p] if lvl == 1 else PTm
            Rc = R0[p] if lvl == 1 else RT
            for h in range(4):
                P.mm(pb[2][:, h * 128:(h + 1) * 128], PTc[:, h, :], Pc[:, h, :])
            if not last:
                for h in range(4):
                    P.mm(pb[3][:, h * 128:(h + 1) * 128], Pc[:, h, :], PTc[:, h, :])
            P.cp("act", f2(Pm), pb[2])
            if not last:
                P.cp("dve", f2(PTm), pb[3])
            yield
            for h in range(4):
                P.mm(pb[6][:, h * 128:(h + 1) * 128], Pm[:, h, :], Rc[:, h, :])
            P.tt("dve", f2(RT), f2(Rc), pb[6], ALU.add)
            yield
        P.cp("act", TTb, RT)
        yield
        for h in range(4):
            P.mm(pb[2][:, h * 128:(h + 1) * 128], TTb[:, h, :], vb[p][:, h, :])
        for h in range(4):
            P.mm(pb[3][:, h * 128:(h + 1) * 128], kbg[p][:, h, :], TTb[:, h, :])
        yield
        P.cp("act", f2(uU), pb[2])
        P.cp("act", f2(wTb), pb[3])
        yield
        for h in range(4):
            P.mm(pb[6][:, h * 128:(h + 1) * 128], wTb[:, h, :], Sb[:, h, :])
        yield
        P.tt("dve", f2(vnew), f2(uU), pb[6], ALU.subtract)
        yield
        if main:
            for h in range(4):
                P.mm(pb[3][:, h * 128:(h + 1) * 128], qgT[p][:, h, :], Sb[:, h, :], start=True, stop=False)
                P.mm(pb[3][:, h * 128:(h + 1) * 128], qkTb[p][:, h, :], vnew[:, h, :], start=False, stop=True)
        for h in range(4):
            P.mm(pb[2][:, h * 128:(h + 1) * 128], kdec[p][:, h, :], vnew[:, h, :])
        yield
        for h in range(4):
            P.stt("dve", Sf[:, h, :], Sf[:, h, :], egl[p][:, h:h + 1], pb[2][:, h * 128:(h + 1) * 128], ALU.mult, ALU.add)
        if t == NPRE - 1:
            P.ts("dve", f2(Sf), f2(Sf), flagT[:, 0:1], ALU.mult)
        P.cp("act", Sb, Sf)
        yield
        if main:
            P.act(f2(osq), pb[3], AF.Square)
            yield
            P.rsum("dve", oss, osq)
            P.rsq(orr, oss, 1.0 / 128)
            yield
            for h in range(4):
                P.stt("dve", ycat[p][:, 512 + h * 128:512 + (h + 1) * 128], pb[3][:, h * 128:(h + 1) * 128],
                      orr[:, h:h + 1], gz[p][:, h, :], ALU.mult, ALU.mult)
            yield
            for k in range(8):
                P.tr(ptp[:, k * 128:(k + 1) * 128], ycat[p][:, k * 128:(k + 1) * 128], identB)
            yield
            P.cp("act", yT, ptp)
            yield
            xo = x1t
            for half in range(2):
                pbo = pb[2 + half]
                for k in range(8):
                    P.mm(pbo, yT[:, k * 128:(k + 1) * 128], w_outB[:, k, half * 512:(half + 1) * 512],
                         start=(k == 0), stop=(k == 7))
            yield
            for half in range(2):
                P.tt("dve", xo[:, half * 512:(half + 1) * 512], pb[2 + half], g1R[:, half * 512:(half + 1) * 512], ALU.mult)
            yield
            P.tt("dve", xo, xo, xs, ALU.add)
            m = t - NPRE
            P.dma("sp", x1_d[m * 128:(m + 1) * 128, :], xo, "x1st")
            if "d_x1" in dbg:
                P.dma("sp", dbg["d_x1"][m * 128:(m + 1) * 128, :], xo, "dbg")
            if "d_ycat" in dbg:
                P.cp("dve", hf, ycat[p])
                P.dma("sp", dbg["d_ycat"][m * 128:(m + 1) * 128, :], hf, "dbg")
            yield

    def interleave(*gs):
        gens = [g for g in gs if g is not None]
        while gens:
            for g in list(gens):
                try:
                    next(g)
                except StopIteration:
                    gens.remove(g)

    def interleave_bal(facts):
        facts = [f for f in facts if f is not None]
        P.dry = True
        totals = [sum(1 for _ in f()) + 1 for f in facts]
        P.dry = False
        gens = [f() for f in facts]
        done = [0] * len(gens)
        live = list(range(len(gens)))
        while live:
            i = min(live, key=lambda i_: (done[i_] + 0.5) / totals[i_])
            try:
                next(gens[i])
            except StopIteration:
                live.remove(i)
            done[i] += 1

    if PIPE:
        for t in range(NT + 1):
            interleave_bal([(lambda t=t: stage1(t)) if t < NT else None,
                            (lambda t=t: gmlp(t)) if (t < NT and t >= NPRE) else None,
                            (lambda t=t: stage2(t - 1)) if t >= 1 else None])
    else:
        for t in range(NT):
            for _ in stage1(t):
                pass
            if t >= NPRE:
                for _ in gmlp(t):
                    pass
            for _ in stage2(t):
                pass

    P.emit()
    s1.close()
    if KSTOP <= 2:
        P.final_wait()
        return nc

    C = cap
    NSLOT = NE * C
    BIG = 1.0e6
    xg_t = nc.dram_tensor("xg", [NSLOT, D], BF16, kind="Internal")
    xg_d = V(xg_t.ap(), [Buf("xg")])
    yy_t = nc.dram_tensor("yy", [NSLOT, D], BF16, kind="Internal")
    yy_d = V(yy_t.ap(), [Buf("yy")])
    I32 = mybir.dt.int32
    U32 = mybir.dt.uint32

    s2p = ExitStack()
    slotI = sb("slotI", [128, NMAIN, 4], I32, st=s2p)
    wk = sb("wk", [128, NMAIN, 4], F32, st=s2p)

    s2 = ExitStack()

    def sb2(name, shape, dt=F32):
        return sb(name, shape, dt, st=s2)

    mod2a = sb2("mod2a", [128, 2 * D])
    P.dma("sp", mod2a, mod2_d[:, 0:2 * D], "mod2a")
    sh2 = mod2a[:, 0:D]
    A2 = mod2a[:, D:2 * D]
    wrF = sb2("wrF", [128, 8, NE])
    P.dma("sp", wrF, w_router.re("(k p) n -> p k n", p=128), "wrF")
    brR = sb2("brR", [128, NE])
    P.dma("sp", brR, bcast_rows(b_router, NE), "brR")
    iotaE = sb2("iotaE", [128, NE])
    P.dma("sp", iotaE, consts2, "iotaE")
    striuB = sb2("striuB", [128, 128], BF16)
    P.tt("dve", striuB, triu, identF, ALU.subtract)
    maskacc = sb2("maskacc", [128, NE], BF16)
    P.memset("dve", maskacc, 0.0)
    def pp(name, shape, dt=F32):
        return [sb2("%s_%d" % (name, i), shape, dt) for i in range(2)]

    slotI_t = [V(slotI.ap[:, s_, :], [Buf("slotI%d" % s_)]) for s_ in range(NMAIN)]
    wk_t = [V(wk.ap[:, s_, :], [Buf("wk%d" % s_)]) for s_ in range(NMAIN)]
    x2 = pp("x2", [128, D])
    h2f_ = pp("h2f", [128, D])
    junk2_ = pp("junk2", [128, D])
    h2b = pp("h2b", [128, D], BF16)
    h2Tf_ = pp("h2Tf", [128, 8, 128])
    ssq2_ = pp("ssq2", [128, 1])
    rstd2_ = pp("rstd2", [128, 1])
    lg_ = pp("lg", [128, NE])
    top8_ = pp("top8", [128, 8])
    idx8_ = pp("idx8", [128, 8], U32)
    ekf_ = pp("ekf", [128, 4])
    msk_ = pp("msk", [128, NE], BF16)
    posS_ = pp("posS", [128, NE])
    tmp32_ = pp("tmp32", [128, NE])
    pk_ = pp("pk", [128, 4])
    val_ = pp("val", [128, 4])
    slotf_ = pp("slotf", [128, 4])
    ex4_ = pp("ex4", [128, 4])
    nmx_ = pp("nmx", [128, 1])
    den_ = pp("den", [128, 1])

    def tile2a(s):
        q = s % 2
        xs, hb2, h2f, junk2, h2Tf = x2[q], h2b[q], h2f_[q], junk2_[q], h2Tf_[q]
        ssq2, rstd2, lg, top8, idx8, ekf, msk = ssq2_[q], rstd2_[q], lg_[q], top8_[q], idx8_[q], ekf_[q], msk_[q]
        posS, tmp32, pk, val, slotf, ex4, nmx, den = posS_[q], tmp32_[q], pk_[q], val_[q], slotf_[q], ex4_[q], nmx_[q], den_[q]
        pA, pB, pC = (pb[0], pb[1], pb[2]) if q == 0 else (pb[4], pb[5], pb[6])
        sI, wkt = slotI_t[s], wk_t[s]
        P.dma("sp", xs, x1_d[s * 128:(s + 1) * 128, :], "x2_%d" % q)
        P.memset("dve", ssq2, 0.0)
        P.act(junk2, xs, AF.Square, accum=ssq2)
        yield
        P.rsq(rstd2, ssq2, 1.0 / D)
        yield
        P.stt("dve", h2f, xs, rstd2, A2, ALU.mult, ALU.mult)
        yield
        P.tt("dve", h2f, h2f, sh2, ALU.add)
        yield
        P.cp("act", hb2, h2f)
        for half, pbh in enumerate((pA, pB)):
            for k in range(4):
                kk = half * 4 + k
                P.tr(pbh[:, k * 128:(k + 1) * 128], h2f[:, kk * 128:(kk + 1) * 128], identF)
        yield
        P.cp("act", h2Tf[:, 0:4, :], f3(pA))
        P.cp("dve", h2Tf[:, 4:8, :], f3(pB))
        yield
        for k in range(8):
            P.mm(pC[:, 0:NE], h2Tf[:, k, :], wrF[:, k, :], start=(k == 0), stop=(k == 7))
        yield
        P.tt("dve", lg, pC[:, 0:NE], brR, ALU.add)
        yield
        P.add("dve", lambda e: e.max(out=top8.ap, in_=lg.ap), [lg], [top8])
        yield
        P.add("dve", lambda e: e.max_index(out=idx8.ap, in_max=top8.ap, in_values=lg.ap), [lg, top8], [idx8])
        P.ts("dve", msk, lg, top8[:, 3:4], ALU.is_ge)
        P.ts("dve", nmx, top8[:, 0:1], -1.0, ALU.mult)
        yield
        P.mm(pC[:, NE:2 * NE], striuB, msk, start=True, stop=False)
        P.mm(pC[:, NE:2 * NE], onesB, maskacc, start=False, stop=True)
        P.tt("dve", maskacc, maskacc, msk, ALU.add)
        P.cp("dve", ekf, idx8[:, 0:4])
        P.act(ex4, top8[:, 0:4], AF.Exp, bias=nmx[:, 0:1])
        yield
        P.cp("act", posS, pC[:, NE:2 * NE])
        P.rsum("dve", den, ex4)
        yield
        P.add("dve", lambda e: e.reciprocal(den.ap, den.ap), [den], [den])
        for k in range(4):
            P.stt("dve", tmp32, iotaE, ekf[:, k:k + 1], posS, ALU.is_equal, ALU.mult)
            P.rsum("dve", pk[:, k:k + 1], tmp32)
            yield
        P.ts("dve", val, pk, float(C), ALU.is_lt)
        P.stt("dve", slotf, ekf, float(C), pk, ALU.mult, ALU.add)
        yield
        P.ts("dve", slotf, slotf, -BIG, ALU.add)
        P.ts("dve", ex4, ex4, den[:, 0:1], ALU.mult)
        yield
        P.tt("dve", slotf, slotf, val, ALU.mult)
        P.tt("dve", wkt, ex4, val, ALU.mult)
        yield
        P.ts("dve", slotf, slotf, BIG, ALU.add)
        yield
        P.cp("dve", sI, slotf)
        yield
        for k in range(4):
            def sc(e, k=k, s=s, hb2=hb2):
                return e.indirect_dma_start(
                    out=xg_d.ap, out_offset=bass.IndirectOffsetOnAxis(ap=slotI[:, s, k:k + 1].ap, axis=0),
                    in_=hb2.ap, in_offset=None, bounds_check=P.bc_reg(e, NSLOT - 1), oob_is_err=False)
            P.add("pool", sc, [sI, hb2], [xg_d], dkey="xgsc%d" % q)
        yield

    for s in range(0, NMAIN, 2):
        interleave(tile2a(s), tile2a(s + 1) if s + 1 < NMAIN else None)
    NSB = C // 512
    flags_t = nc.dram_tensor("flags", [1, NSB * NE], I32, kind="Internal")
    flags_d = V(flags_t.ap(), [Buf("flags")])
    P.flags_ap = flags_t.ap()
    P.mm(pb[3][:, 0:NE], onesB, maskacc)
    cntS = sb2("cntS", [128, NE])
    P.cp("act", cntS, pb[3][:, 0:NE])
    flagf = sb2("flagf", [128, NSB * NE])
    for sbk in range(NSB):
        P.ts("dve", flagf[:, sbk * NE:(sbk + 1) * NE], cntS, float(sbk * 512), ALU.is_gt)
    flagt = sb2("flagt", [128, NE])
    P.cp("dve", flagt, flagf[:, 0:NE])
    P.tt("dve", flagf[:, 0:NE - 1], flagt[:, 0:NE - 1], flagt[:, 1:NE], ALU.max)
    flagI = sb2("flagI", [128, NSB * NE], I32)
    P.cp("dve", flagI, flagf)
    P.dma("sp", flags_d, flagI[0:1, :], "flags")
    P.emit()
    s2.close()

    s2 = ExitStack()
    bguT = sb2("bguT", [128, 16, NE])
    s2t = ExitStack()
    bguRows = sb("bguRows", [NE, 2 * D], F32, st=s2t)
    P.dma("sp", bguRows, b_gu, "bguRows")
    for c in range(16):
        P.tr(pb[c % 2][:, 0:NE], bguRows[:, c * 128:(c + 1) * 128], identF[0:NE, 0:NE])
        P.cp("dve", bguT[:, c, :], pb[c % 2][:, 0:NE])
    P.emit()
    s2t.close()
    ones1 = sb2("ones1", [1, 128], BF16)
    P.memset("dve", ones1, 1.0)
    wguB = [sb2("wguB%d" % i, [128, 8, 2 * D], BF16) for i in range(2)]
    wdB = [sb2("wdB%d" % i, [128, 8, D], BF16) for i in range(2)]
    bdr = [sb2("bdr%d" % i, [1, D], BF16) for i in range(2)]
    xr = [sb2("xr%d" % i, [128, D], BF16) for i in range(4)]
    xgT = [sb2("xgT%d" % i, [128, 8, 512], BF16) for i in range(2)]
    actT = [sb2("actT%d" % i, [128, 8, 512], BF16) for i in range(2)]
    actK = [[V(a_.ap[:, k_, :], [Buf("actT%d_%d" % (i_, k_))]) for k_ in range(8)] for i_, a_ in enumerate(actT)]
    gq = [sb2("gq%d" % i, [128, 512]) for i in range(2)]
    uq = [sb2("uq%d" % i, [128, 512]) for i in range(2)]
    sg = sb2("sg0", [128, 512])
    tq = sb2("tq0", [128, 512])
    ysb = [sb2("ysb%d" % i, [128, D], BF16) for i in range(2)]

    w_gu_v = w_gu.re("e (k p) n -> e p k n", p=128)
    w_down_v = w_down.re("e (k p) n -> e p k n", p=128)

    stg = [sb2("stg%d" % i, [128, 2048]) for i in range(2)]

    def load_expert(e, slot):
        for k in range(0, 4, 2):
            P.dma("pool", wguB[slot][:, k:k + 2, :], w_gu_v[e, :, k:k + 2, :], "wgu%d" % slot)
        P.dma("pool", bdr[slot], b_down[e:e + 1, :], "bdr%d" % slot)

    def staged_chunks(e, slot):
        ch = []
        for k in range(4, 8):
            ch.append((w_gu_v[e, :, k, :], wguB[slot][:, k, :], False))
        for k in range(0, 8, 2):
            ch.append((w_down_v[e, :, k:k + 2, :], wdB[slot][:, k:k + 2, :], True))
        return ch

    def stream_steps(e, slot):
        ch = staged_chunks(e, slot)
        n = len(ch)
        acts = []
        for c in range(n + 2):
            def act_(c=c):
                if 0 <= c - 2 < n:
                    src, dst, two = ch[c - 2]
                    i = (c - 2) % 2
                    sv = stg[i].re("p (a n) -> p a n", a=2) if two else stg[i]
                    P.cp("dve", dst, sv)
                if c < n:
                    src, dst, two = ch[c]
                    i = c % 2
                    sv = stg[i].re("p (a n) -> p a n", a=2) if two else stg[i]
                    P.dma("act", sv, src, "stg%d" % i)
            acts.append(act_)
        return acts

    load_expert(0, 0)
    for a_ in stream_steps(0, 0):
        a_()
    nblk = [0]
    nsb = 0

    def row_loads(e, sbk):
        r0_ = e * C + sbk * 512
        rows = []
        for j in range(4):
            xrow = xr[nblk[0] % 4]
            P.dma("sp", xrow, xg_d[r0_ + j * 128:r0_ + (j + 1) * 128, :], "xr%d" % (nblk[0] % 4))
            nblk[0] += 1
            rows.append(xrow)
        return rows

    def row_T(rows, j, xT_):
        for k in range(8):
            P.tr(ptp[:, k * 128:(k + 1) * 128], rows[j][:, k * 128:(k + 1) * 128], identB)
        P.cp("act" if j % 2 == 0 else "dve", xT_[:, :, j * 128:(j + 1) * 128], ptp.re("p (k t) -> p k t", k=8))

    P.cond = (0,) if use_skip else None
    rows0 = row_loads(0, 0)
    for j in range(4):
        row_T(rows0, j, xgT[0])
    P.cond = None
    for e in range(n_exp):
        slot = e % 2
        bsteps = []
        if e + 1 < n_exp:
            load_expert(e + 1, 1 - slot)
            bsteps = stream_steps(e + 1, 1 - slot)
        wg = wguB[slot]
        wd = wdB[slot]
        xT = xgT[e % 2]
        for sbk in range(C // 512):
            if use_skip:
                P.cond = (e,) if sbk == 0 else tuple(k_ * NE + e for k_ in range(1, sbk + 1))
            else:
                P.cond = None
            aT = actT[nsb % 2]
            aK = actK[nsb % 2]
            nsb += 1
            r0 = e * C + sbk * 512
            nxt = None
            if sbk == 0:
                if e + 1 < n_exp:
                    nxt = row_loads(e + 1, 0)
            else:
                rws = row_loads(e, sbk)
                for j in range(4):
                    row_T(rws, j, xT)
            for fc in range(8):
                pg_ = pb[fc % 2]
                pu_ = pb[2 + fc % 2]
                for k in range(8):
                    P.mm(pg_, wg[:, k, fc * 128:(fc + 1) * 128], xT[:, k, :], start=(k == 0), stop=(k == 7))
                for k in range(8):
                    P.mm(pu_, wg[:, k, D + fc * 128:D + (fc + 1) * 128], xT[:, k, :], start=(k == 0), stop=(k == 7))
                i2 = fc % 2
                P.ts("dve", gq[i2], pg_, bguT[:, fc, e:e + 1], ALU.add, 7.0, ALU.min)
                P.act(sg, gq[i2], AF.Sigmoid, scale=1.702)
                P.ts("dve", uq[i2], pu_, bguT[:, 8 + fc, e:e + 1], ALU.add, 7.0, ALU.min)
                P.ts("dve", uq[i2], uq[i2], -7.0, ALU.max, 1.0, ALU.add)
                P.tt("dve" if fc == 7 else "pool", tq, gq[i2], sg, ALU.mult)
                P.tt("dve" if fc == 7 else "pool", aK[fc], tq, uq[i2], ALU.mult)
                for _ in range(2 if fc >= 6 else 1):
                    if bsteps:
                        P.always = True
                        bsteps.pop(0)()
                        P.always = False
            for j in range(4):
                if nxt is not None:
                    row_T(nxt, j, xgT[(e + 1) % 2])
                yb_ = ysb[j % 2]
                for half in range(2):
                    py = pb[4 + half]
                    for k in range(8):
                        P.mm(py, aK[k][:, j * 128:(j + 1) * 128], wd[:, k, half * 512:(half + 1) * 512],
                             start=(k == 0), stop=False)
                    P.mm(py, ones1, bdr[slot][:, half * 512:(half + 1) * 512], start=False, stop=True)
                    P.cp("act", yb_[:, half * 512:(half + 1) * 512], py)
                P.dma("sp", yy_d[r0 + j * 128:r0 + (j + 1) * 128, :], yb_, "yst%d" % (j % 2))
            P.cond = None
        while bsteps:
            bsteps.pop(0)()
    P.emit()
    s2.close()

    s2 = ExitStack()
    yg = [[sb2("yg%d_%d" % (i, k), [128, D], BF16) for k in range(4)] for i in range(2)]
    for i in range(2):
        for k in range(4):
            P.memset("dve" if k % 2 == 0 else "pool", yg[i][k], 0.0)
    g2R = sb2("g2R", [128, D])
    P.dma("sp", g2R, mod2_d[:, 2 * D:3 * D], "g2R")
    nfR = sb2("nfR", [128, D])
    P.dma("sp", nfR, bcast_rows(norm_f_w, D), "nfR")
    x2 = [sb2("x2c_%d" % i, [128, D]) for i in range(2)]
    acc_ = [sb2("acc%d" % i, [128, D]) for i in range(2)]
    xo2_ = [sb2("xo2_%d" % i, [128, D]) for i in range(2)]
    junk2_ = [sb2("junk2c%d" % i, [128, D]) for i in range(2)]
    ssq2_ = [sb2("ssq2c%d" % i, [128, 1]) for i in range(2)]
    rstd2_ = [sb2("rstd2c%d" % i, [128, 1]) for i in range(2)]

    def tile2c(s):
        q = s % 2
        xs, ygs, acc, xo, junk2, ssq2, rstd2 = x2[q], yg[q], acc_[q], xo2_[q], junk2_[q], ssq2_[q], rstd2_[q]
        sI, wkt = slotI_t[s], wk_t[s]
        P.dma("sp", xs, x1_d[s * 128:(s + 1) * 128, :], "x2c_%d" % q)
        for k in range(4):
            def ga(e, k=k, s=s, ygs=ygs):
                return e.indirect_dma_start(
                    out=ygs[k].ap, out_offset=None, in_=yy_d.ap,
                    in_offset=bass.IndirectOffsetOnAxis(ap=slotI[:, s, k:k + 1].ap, axis=0),
                    bounds_check=P.bc_reg(e, NSLOT - 1), oob_is_err=False)
            P.add("pool", ga, [sI, yy_d], [ygs[k]], dkey="yg%d_%d" % (q, k))
        yield
        P.ts("dve", acc, ygs[0], wkt[:, 0:1], ALU.mult)
        yield
        for k in range(1, 4):
            P.stt("dve", acc, ygs[k], wkt[:, k:k + 1], acc, ALU.mult, ALU.add)
            yield
        P.tt("dve", xo, acc, g2R, ALU.mult)
        yield
        P.tt("dve", xo, xo, xs, ALU.add)
        P.memset("dve", ssq2, 0.0)
        yield
        P.act(junk2, xo, AF.Square, accum=ssq2)
        yield
        P.rsq(rstd2, ssq2, 1.0 / D)
        yield
        P.stt("dve", xo, xo, rstd2, nfR, ALU.mult, ALU.mult)
        yield
        P.dma("sp", out_d[s * 128:(s + 1) * 128, :], xo, "out%d" % q)
        yield

    for s in range(0, NMAIN, 2):
        interleave(tile2c(s), tile2c(s + 1) if s + 1 < NMAIN else None)
    P.emit()
    P.final_wait()
    s2.close()
    s2p.close()
    es.close()
    return nc


def make_consts():
    c = np.zeros((128, 512 + NE), np.float32)
    c[:, 512:] = np.arange(NE, dtype=np.float32)[None, :]
    i = np.arange(128)
    c[:, 0:128] = np.eye(128, dtype=np.float32)
    c[:, 128:256] = (i[None, :] >= i[:, None])
    c[:, 256:384] = (i[None, :] <= i[:, None])
    c[:, 384:512] = (i[None, :] < i[:, None])
    return c


def core_inputs(inp, b, half, npre, nmain):
    x = inp["x"]
    L = 0
    f = np.ascontiguousarray
    if half == 0:
        xs = np.concatenate([x[b, :npre * 128], x[b, :nmain * 128]], axis=0)
        flag = 0.0
    else:
        xs = x[b, :(npre + nmain) * 128]
        flag = 1.0
    m = {
        "x_seq": f(xs),
        "flag": np.full((128, 1), flag, np.float32),
        "c_row": f(inp["c"][b].reshape(8, 128)),
        "consts": make_consts(),
        "w_ada": f(inp["w_ada"][L]),
        "b_ada": f(inp["b_ada"][L].reshape(1, -1)),
        "norm1_w": f(inp["norm1_w"][L].reshape(1, -1)),
        "w_in": f(inp["w_in"][L]),
        "gm_vnorm_w": f(inp["gm_vnorm_w"][L].reshape(1, -1)),
        "gm_w_spatial": f(inp["gm_w_spatial"][L]),
        "gm_b_spatial": f(inp["gm_b_spatial"][L]),
        "gdn_conv_w": f(inp["gdn_conv_w"][L]),
        "gdn_a_log": f(inp["gdn_a_log"][L].reshape(1, -1)),
        "gdn_dt_bias": f(inp["gdn_dt_bias"][L].reshape(1, -1)),
        "gdn_onorm_w": f(inp["gdn_onorm_w"][L].reshape(1, -1)),
        "w_out": f(inp["w_out"][L]),
        "norm2_w": f(inp["norm2_w"][L].reshape(1, -1)),
        "w_router": f(inp["w_router"][L]),
        "b_router": f(inp["b_router"][L].reshape(1, -1)),
        "w_gu": f(inp["w_gu"][L]),
        "b_gu": f(inp["b_gu"][L]),
        "w_down": f(inp["w_down"][L]),
        "b_down": f(inp["b_down"][L]),
        "norm_f_w": f(inp["norm_f_w"].reshape(1, -1)),
    }
    return m


def kernel(**inputs):
    inp = {k: np.asarray(v, dtype=np.float32) for k, v in inputs.items()}
    B, S, _ = inp["x"].shape
    nh = S // 2 // 128
    nc = build(nh, nh)
    in_maps = []
    for c in range(8):
        in_maps.append(core_inputs(inp, c // 2, c % 2, nh, nh))
    res = run_bass_kernel_spmd(nc, in_maps, core_ids=list(range(8)))
    out = np.empty((B, S, D), np.float32)
    for c in range(8):
        b, half = c // 2, c % 2
        out[b, half * (S // 2):(half + 1) * (S // 2)] = res.results[c]["out"]
    return out
```
